# Optimizing a Trainium2 kernel written in Bass

```python
import jax, jax.numpy as jnp
from jax import lax
import numpy as np

D_MODEL = 1024
BATCH = 4
SEQ = 8192
DEPTH = 4

BRANCH_WIDTH = D_MODEL // 2
N_BRANCHES = 4
ATT_HEADS = 8
ATT_KV_HEADS = 2
ATT_HEAD_DIM = BRANCH_WIDTH // ATT_HEADS
ROT_DIM = ATT_HEAD_DIM // 4
ROPE_THETA = 500000.0
IDX_HEADS = 4
IDX_HEAD_DIM = 64
INDEX_TOPK = 256
Q_BLOCK = 128
RWKV_HEAD = 64
RWKV_HEADS = BRANCH_WIDTH // RWKV_HEAD
RWKV_DECAY_LORA = 32
RWKV_A_LORA = 32
RWKV_GATE_LORA = 96
RWKV_GN_EPS = 64e-5
POOL_WINDOWS = (2, 4, 8, 16)
POOL_GROUPS = len(POOL_WINDOWS)
POOL_GROUP_DIM = BRANCH_WIDTH // POOL_GROUPS
RET_HEADS = 8
RET_KEY_DIM = BRANCH_WIDTH // 2 // RET_HEADS
RET_VALUE_DIM = BRANCH_WIDTH // RET_HEADS
RET_CHUNK = 128
RET_THETA = 10000.0
RET_GN_EPS = 1e-6
D_FF = 4 * D_MODEL
NORM_EPS = 1e-5

A_COLS = (ATT_HEADS * ATT_HEAD_DIM, ATT_KV_HEADS * ATT_HEAD_DIM, ATT_KV_HEADS * ATT_HEAD_DIM,
          IDX_HEADS * IDX_HEAD_DIM, IDX_HEAD_DIM, IDX_HEADS)
B_COLS = (BRANCH_WIDTH, RWKV_DECAY_LORA, BRANCH_WIDTH, BRANCH_WIDTH, RWKV_A_LORA, RWKV_GATE_LORA)
C_COLS = (BRANCH_WIDTH,)
D_COLS = (RET_HEADS * RET_KEY_DIM, RET_HEADS * RET_KEY_DIM, RET_HEADS * RET_VALUE_DIM, BRANCH_WIDTH)
N_A = sum(A_COLS)
N_B = sum(B_COLS)
N_C = sum(C_COLS)
N_D = sum(D_COLS)
N_G = N_BRANCHES * D_MODEL
N_IN = N_A + N_B + N_C + N_D + N_G

kernel_name = 'hybrid_gated_dsa_rwkv7_pool_retention'


def rms_norm(x, g):
    xf = x.astype(jnp.float32)
    y = xf * lax.rsqrt(jnp.mean(xf * xf, -1, keepdims=True) + NORM_EPS)
    return (y * g.astype(jnp.float32)).astype(x.dtype)


def split_cols(p, sizes):
    outs, off = [], 0
    for sz in sizes:
        outs.append(p[..., off:off + sz])
        off += sz
    return outs


def cos_sin(positions, inv_freq, dtype):
    ang = positions.astype(jnp.float32)[..., None] * inv_freq
    return jnp.cos(ang).astype(dtype)[:, :, None, :], jnp.sin(ang).astype(dtype)[:, :, None, :]


def rotate(x, cos, sin):
    half = x.shape[-1] // 2
    x1, x2 = x[..., :half], x[..., half:]
    return jnp.concatenate([x1 * cos - x2 * sin, x2 * cos + x1 * sin], -1)


def partial_rope(x, cos, sin):
    return jnp.concatenate([rotate(x[..., :ROT_DIM], cos, sin), x[..., ROT_DIM:]], -1)


def dsa_attention(q, k, v, q_idx, k_idx, w_idx):
    b, s = q.shape[0], q.shape[1]
    top_k = min(INDEX_TOPK, s // 4)
    rep = ATT_HEADS // ATT_KV_HEADS
    scale = ATT_HEAD_DIM ** -0.5
    key_pos = jnp.arange(s)
    gather = jax.vmap(lambda kb, ib: kb[ib])

    def one_block(i):
        start = i * Q_BLOCK
        qb = lax.dynamic_slice_in_dim(q, start, Q_BLOCK, axis=1)
        qib = lax.dynamic_slice_in_dim(q_idx, start, Q_BLOCK, axis=1)
        wib = lax.dynamic_slice_in_dim(w_idx, start, Q_BLOCK, axis=1)
        qpos = start + jnp.arange(Q_BLOCK)
        causal = key_pos[None, :] <= qpos[:, None]
        idx_s = jax.nn.relu(jnp.einsum('bqhd,bsd->bqhs', qib, k_idx))
        idx_s = jnp.einsum('bqhs,bqh->bqs', idx_s, wib).astype(jnp.float32)
        idx_s = jnp.where(causal[None], idx_s, -jnp.inf)
        _, sel = lax.top_k(idx_s, top_k)
        valid = sel <= qpos[None, :, None]
        kg = gather(k, sel)
        vg = gather(v, sel)
        qg = qb.reshape(b, Q_BLOCK, ATT_KV_HEADS, rep, ATT_HEAD_DIM)
        logits = jnp.einsum('bqgrd,bqkgd->bqgrk', qg, kg).astype(jnp.float32) * scale
        logits = jnp.where(valid[:, :, None, None, :], logits, -jnp.inf)
        p = jax.nn.softmax(logits, axis=-1).astype(v.dtype)
        o = jnp.einsum('bqgrk,bqkgd->bqgrd', p, vg)
        return o.reshape(b, Q_BLOCK, ATT_HEADS * ATT_HEAD_DIM)

    out = lax.map(one_block, jnp.arange(s // Q_BLOCK))
    return jnp.swapaxes(out, 0, 1).reshape(b, s, ATT_HEADS * ATT_HEAD_DIM)


def token_shift(p, mu):
    prev = jnp.pad(p, ((0, 0), (1, 0), (0, 0)))[:, :-1]
    return p + (prev - p) * mu


def rwkv7_time_mix(p, mu, w0, w2, a0, a2, g2, k_k, k_a, r_k, ln_g, ln_b):
    b, s, _ = p.shape
    f32 = jnp.float32
    p = token_shift(p, mu)
    r, wd, k, v, ad, gd = split_cols(p, B_COLS)
    w = -jax.nn.softplus(-(w0 + jnp.tanh(wd) @ w2)) - 0.5
    a = jax.nn.sigmoid(a0 + ad @ a2)
    g = jax.nn.sigmoid(gd) @ g2
    heads = lambda t: t.astype(f32).reshape(b, s, RWKV_HEADS, RWKV_HEAD)
    hv = lambda t: t.astype(f32).reshape(RWKV_HEADS, RWKV_HEAD)
    r, w, k, v, a = heads(r), heads(w), heads(k), heads(v), heads(a)
    kk = k * hv(k_k)
    kk = kk / jnp.maximum(jnp.sqrt(jnp.sum(kk * kk, -1, keepdims=True)), 1e-12)
    k = k * (1.0 + (a - 1.0) * hv(k_a))
    decay = jnp.exp(-jnp.exp(w))

    def step(state, inp):
        r_t, d_t, k_t, v_t, kk_t, b_t = inp
        sk = jnp.einsum('bhij,bhj->bhi', state, kk_t)
        state = (state * d_t[:, :, None, :] - sk[..., None] * b_t[:, :, None, :]
                 + v_t[..., None] * k_t[:, :, None, :])
        return state, jnp.einsum('bhij,bhj->bhi', state, r_t)

    tm = lambda t: jnp.swapaxes(t, 0, 1)
    state0 = jnp.zeros((b, RWKV_HEADS, RWKV_HEAD, RWKV_HEAD), f32)
    _, y = lax.scan(step, state0, (tm(r), tm(decay), tm(k), tm(v), tm(kk), tm(kk * a)))
    y = jnp.swapaxes(y, 0, 1)
    mean = jnp.mean(y, -1, keepdims=True)
    var = jnp.mean(jnp.square(y - mean), -1, keepdims=True)
    y = (y - mean) * lax.rsqrt(var + RWKV_GN_EPS) * hv(ln_g) + hv(ln_b)
    y = y + jnp.sum(r * k * hv(r_k), -1, keepdims=True) * v
    return y.reshape(b, s, BRANCH_WIDTH).astype(p.dtype) * g


def multiscale_pool(p, pool_w, pool_scale):
    b, s, _ = p.shape
    xf = p.astype(jnp.float32)
    csum = jnp.pad(jnp.cumsum(xf, axis=1), ((0, 0), (1, 0), (0, 0)))
    steps = jnp.arange(1, s + 1, dtype=jnp.float32)
    groups = []
    for gi, win in enumerate(POOL_WINDOWS):
        lo, hi = gi * POOL_GROUP_DIM, (gi + 1) * POOL_GROUP_DIM
        c = csum[:, :, lo:hi]
        lagged = jnp.pad(c, ((0, 0), (win, 0), (0, 0)))[:, 1:s + 1]
        mean = (c[:, 1:] - lagged) / jnp.minimum(steps, float(win))[None, :, None]
        groups.append(mean - xf[:, :, lo:hi])
    pooled = jnp.stack(groups, axis=2).astype(p.dtype)
    y = jnp.einsum('bsgc,gcd->bsgd', pooled, pool_w).reshape(b, s, BRANCH_WIDTH)
    return y * pool_scale


def retention(q, k, v, g, gn_g):
    b, s, h, dk = q.shape
    dv = v.shape[-1]
    f32 = jnp.float32
    nc = s // RET_CHUNK
    log_gamma = jnp.log(1.0 - 2.0 ** (-5.0 - jnp.arange(h, dtype=f32)))
    pos = jnp.arange(RET_CHUNK, dtype=f32)
    diff = pos[:, None] - pos[None, :]
    intra = jnp.where(diff >= 0, jnp.exp(jnp.maximum(diff, 0.0)[None] * log_gamma[:, None, None]), 0.0)
    q_decay = jnp.exp((pos + 1.0)[None, :] * log_gamma[:, None])
    k_decay = jnp.exp((RET_CHUNK - 1.0 - pos)[None, :] * log_gamma[:, None])
    c_decay = jnp.exp(RET_CHUNK * log_gamma)
    chunk = lambda t: t.astype(f32).reshape(b, nc, RET_CHUNK, h, t.shape[-1]).transpose(1, 0, 3, 2, 4)
    qc, kc, vc = chunk(q), chunk(k * dk ** -0.5), chunk(v)

    def step(state, inp):
        qj, kj, vj = inp
        sc = jnp.einsum('bhnd,bhmd->bhnm', qj, kj) * intra
        o = (jnp.einsum('bhnm,bhmv->bhnv', sc, vj)
             + jnp.einsum('bhnd,bhdv->bhnv', qj, state) * q_decay[..., None])
        state = state * c_decay[:, None, None] + jnp.einsum('bhmd,bhmv->bhdv', kj * k_decay[..., None], vj)
        return state, o

    _, o = lax.scan(step, jnp.zeros((b, h, dk, dv), f32), (qc, kc, vc))
    o = o.transpose(1, 0, 3, 2, 4).reshape(b, s, h, dv)
    mean = jnp.mean(o, -1, keepdims=True)
    var = jnp.mean(jnp.square(o - mean), -1, keepdims=True)
    o = ((o - mean) * lax.rsqrt(var + RET_GN_EPS)).reshape(b, s, h * dv) * gn_g.astype(f32)
    return jax.nn.silu(g) * o.astype(g.dtype)


def setup_inputs(seed: int = 0) -> dict:
    key = jax.random.key(seed)
    ks = jax.random.split(key, 32)
    f32 = jnp.float32
    nrm = lambda kk, shape, sc: jax.random.normal(kk, shape, f32) * sc
    L, D, W = DEPTH, D_MODEL, BRANCH_WIDTH
    return {
        'x': nrm(ks[0], (BATCH, SEQ, D), 1.0),
        'positions': jnp.tile(jnp.arange(SEQ, dtype=jnp.int32)[None, :], (BATCH, 1)),
        'attn_norm_g': 1.0 + nrm(ks[1], (L, D), 0.05),
        'w_in': nrm(ks[2], (L, D, N_IN), D ** -0.5),
        'rwkv_mu': jax.random.uniform(ks[3], (L, N_B), f32, 0.2, 0.8),
        'rwkv_w0': jnp.linspace(-6.0, -1.0, W, dtype=f32)[None, :] + nrm(ks[4], (L, W), 0.1),
        'rwkv_w2': nrm(ks[5], (L, RWKV_DECAY_LORA, W), 0.5 * RWKV_DECAY_LORA ** -0.5),
        'rwkv_a0': nrm(ks[6], (L, W), 0.1),
        'rwkv_a2': nrm(ks[7], (L, RWKV_A_LORA, W), RWKV_A_LORA ** -0.5),
        'rwkv_g2': nrm(ks[8], (L, RWKV_GATE_LORA, W), RWKV_GATE_LORA ** -0.5),
        'rwkv_k_k': 0.85 + nrm(ks[9], (L, W), 0.05),
        'rwkv_k_a': 1.0 + nrm(ks[10], (L, W), 0.05),
        'rwkv_r_k': nrm(ks[11], (L, W), 0.1),
        'rwkv_ln_g': 1.0 + nrm(ks[12], (L, W), 0.05),
        'rwkv_ln_b': nrm(ks[13], (L, W), 0.05),
        'pool_w': nrm(ks[14], (L, POOL_GROUPS, POOL_GROUP_DIM, POOL_GROUP_DIM), POOL_GROUP_DIM ** -0.5),
        'pool_scale': 1.0 + nrm(ks[15], (L, W), 0.1),
        'ret_gn_g': 1.0 + nrm(ks[16], (L, W), 0.05),
        'gate_b': nrm(ks[17], (L, N_BRANCHES, D), 0.1),
        'w_branch': nrm(ks[18], (L, N_BRANCHES, W, D), W ** -0.5),
        'w_out': nrm(ks[19], (L, D, D), D ** -0.5),
        'mlp_norm_g': 1.0 + nrm(ks[20], (L, D), 0.05),
        'mlp_up': nrm(ks[21], (L, D, D_FF), D ** -0.5),
        'mlp_down': nrm(ks[22], (L, D_FF, D), D_FF ** -0.5),
        'final_norm_g': 1.0 + nrm(ks[23], (D,), 0.05),
    }


def reference(x, positions, attn_norm_g, w_in, rwkv_mu, rwkv_w0, rwkv_w2, rwkv_a0, rwkv_a2, rwkv_g2,
              rwkv_k_k, rwkv_k_a, rwkv_r_k, rwkv_ln_g, rwkv_ln_b, pool_w, pool_scale, ret_gn_g,
              gate_b, w_branch, w_out, mlp_norm_g, mlp_up, mlp_down, final_norm_g):
    b, s, _ = x.shape
    dt = x.dtype
    inv_freq_att = ROPE_THETA ** (-jnp.arange(0, ROT_DIM, 2, dtype=jnp.float32) / ROT_DIM)
    cos_a, sin_a = cos_sin(positions, inv_freq_att, dt)
    inv_freq_ret = 1.0 / (RET_THETA ** jnp.linspace(0.0, 1.0, RET_KEY_DIM // 2, dtype=jnp.float32))
    cos_r, sin_r = cos_sin(positions, inv_freq_ret, dt)
    for l in range(DEPTH):
        h = rms_norm(x, attn_norm_g[l])
        proj = h @ w_in[l]
        pa, pb, pc, pd, pg = split_cols(proj, (N_A, N_B, N_C, N_D, N_G))
        q, k, v, qi, ki, wi = split_cols(pa, A_COLS)
        q = partial_rope(q.reshape(b, s, ATT_HEADS, ATT_HEAD_DIM), cos_a, sin_a)
        k = partial_rope(k.reshape(b, s, ATT_KV_HEADS, ATT_HEAD_DIM), cos_a, sin_a)
        v = v.reshape(b, s, ATT_KV_HEADS, ATT_HEAD_DIM)
        qi = partial_rope(qi.reshape(b, s, IDX_HEADS, IDX_HEAD_DIM), cos_a, sin_a)
        ki = partial_rope(ki[:, :, None, :], cos_a, sin_a)[:, :, 0]
        o_a = dsa_attention(q, k, v, qi, ki, wi)
        o_b = rwkv7_time_mix(pb, rwkv_mu[l], rwkv_w0[l], rwkv_w2[l], rwkv_a0[l], rwkv_a2[l], rwkv_g2[l],
                             rwkv_k_k[l], rwkv_k_a[l], rwkv_r_k[l], rwkv_ln_g[l], rwkv_ln_b[l])
        o_c = multiscale_pool(pc, pool_w[l], pool_scale[l])
        rq, rk, rv, rg = split_cols(pd, D_COLS)
        rq = rotate(rq.reshape(b, s, RET_HEADS, RET_KEY_DIM), cos_r, sin_r)
        rk = rotate(rk.reshape(b, s, RET_HEADS, RET_KEY_DIM), cos_r, sin_r)
        o_d = retention(rq, rk, rv.reshape(b, s, RET_HEADS, RET_VALUE_DIM), rg, ret_gn_g[l])
        gates = jax.nn.sigmoid(pg.reshape(b, s, N_BRANCHES, D_MODEL) + gate_b[l])
        merged = gates[:, :, 0] * (o_a @ w_branch[l, 0])
        merged = merged + gates[:, :, 1] * (o_b @ w_branch[l, 1])
        merged = merged + gates[:, :, 2] * (o_c @ w_branch[l, 2])
        merged = merged + gates[:, :, 3] * (o_d @ w_branch[l, 3])
        x = x + merged @ w_out[l]
        h2 = rms_norm(x, mlp_norm_g[l])
        x = x + jnp.square(jax.nn.relu(h2 @ mlp_up[l])) @ mlp_down[l]
    return rms_norm(x, final_norm_g)
```

```python
import math
from contextlib import ExitStack

import numpy as np
import concourse.bass as bass
import concourse.mybir as mybir
from concourse.bass_utils import run_bass_kernel_spmd

F32 = mybir.dt.float32
BF16 = mybir.dt.bfloat16
I32 = mybir.dt.int32
ALU = mybir.AluOpType
AF = mybir.ActivationFunctionType
AX = mybir.AxisListType

D = 1024
NCH = 8
W = 512
NB = 4
NORM_EPS = 1e-5
N_A, N_B, N_C, N_D = 1092, 1696, 512, 1536
N_G = 4096
WA_COLS = 1156
POOL_WINDOWS = (2, 4, 8, 16)
PV_AG, PV_MG, PV_GB, PV_PS = 0, 8, 16, 48
NPV = 64
RV_GNG, RV_W0, RV_A0, RV_KK, RV_KA, RV_RK, RV_LNG, RV_LNB = range(8)
NROW = 12
RWKV_GN_EPS = 64e-5
RET_GN_EPS = 1e-6


def colmajor(v):
    v = np.asarray(v, dtype=np.float32)
    return np.ascontiguousarray(v.reshape(-1, 128).T)


class Tk:
    __slots__ = ("w", "r")

    def __init__(self):
        self.w = None
        self.r = {}


class Buf:
    def __init__(self, t):
        self.t = t
        self.k = Tk()

    def __getitem__(self, idx):
        return self.t[idx]


class HView(Buf):
    def __init__(self, buf):
        self.t = buf.t
        self.k = buf.k

    def __getitem__(self, idx):
        p, c, sl = idx
        a = 0 if sl.start is None else sl.start
        b = 512 if sl.stop is None else sl.stop
        return self.t[p, c, 8 + a:8 + b]


class Eng:
    def __init__(self, kb, name, handle, is_pe=False):
        self.kb = kb
        self.name = name
        self.h = handle
        self.is_pe = is_pe
        self.sem = kb.new_sem(name)
        self.n = 0
        self.seen = {}
        self.cnt = 0

    def need(self, tok):
        if tok is None:
            return False
        sem, val = tok
        if self.is_pe and sem is self.sem:
            return False
        return self.seen.get(id(sem), 0) < val

    def mark(self, tok):
        self.seen[id(tok[0])] = max(self.seen.get(id(tok[0]), 0), tok[1])


class KB:
    def __init__(self, nc):
        self.nc = nc
        self.es = ExitStack()
        self.stack = [self.es]
        self.nsem = 0
        self.E = {
            "pe": Eng(self, "pe", nc.tensor, is_pe=True),
            "dve": Eng(self, "dve", nc.vector),
            "act": Eng(self, "act", nc.scalar),
            "pool": Eng(self, "pool", nc.gpsimd),
            "sp": Eng(self, "sp", nc.sync),
        }
        self.dsq = {"sp": [[self.new_sem("dsp%d" % i), 0] for i in range(16)],
                    "pool": [[self.new_sem("dpl%d" % i), 0] for i in range(8)],
                    "act": [[self.new_sem("dac%d" % i), 0] for i in range(4)]}
        self.dsems = [x for v in self.dsq.values() for x in v]
        self.dnext = {"sp": 0, "pool": 0, "act": 0}
        self.ninst = 0
        self.dram_tk = {}

    def new_sem(self, name):
        self.nsem += 1
        return self.es.enter_context(self.nc.semaphore("s_%s_%d" % (name, self.nsem)))

    def sb(self, name, shape, dt):
        self.nname = getattr(self, "nname", 0) + 1
        return Buf(self.stack[-1].enter_context(self.nc.sbuf_tensor("%s_%d" % (name, self.nname), list(shape), dt)))

    def ps(self, name, shape, dt=F32):
        self.nname = getattr(self, "nname", 0) + 1
        b = Buf(self.stack[-1].enter_context(self.nc.psum_tensor("%s_%d" % (name, self.nname), list(shape), dt)))
        b.psum = True
        return b

    def barrier(self):
        toks = [(o.sem, o.n) for o in self.E.values() if o.n > 0]
        toks += [(ds[0], ds[1]) for ds in self.dsems if ds[1] > 0]
        for e in self.E.values():
            for tok in toks:
                if tok[0] is e.sem:
                    continue
                if e.need(tok):
                    e.h.wait_ge(tok[0], tok[1])
                    e.mark(tok)
                    self.ninst += 1

    def scope(self):
        kb = self

        class _Sc:
            def __enter__(self_):
                self_.es = ExitStack()
                kb.stack.append(self_.es)

            def __exit__(self_, *a):
                kb.barrier()
                kb.stack.pop()
                self_.es.close()
                return False

        return _Sc()

    def dk(self, key):
        if key not in self.dram_tk:
            self.dram_tk[key] = Tk()
        return self.dram_tk[key]

    @staticmethod
    def _tk(x):
        return x.k if isinstance(x, Buf) else x

    def _collect(self, e, R, Wr):
        toks = {}

        def add(tok):
            if e.need(tok):
                k = id(tok[0])
                if k not in toks or toks[k][1] < tok[1]:
                    toks[k] = tok

        for t in R:
            add(self._tk(t).w)
            if getattr(t, "psum", False):
                for tok in self._tk(t).r.values():
                    if tok[0] is not e.sem:
                        add(tok)
        for t in Wr:
            tk = self._tk(t)
            add(tk.w)
            for tok in tk.r.values():
                add(tok)
        return list(toks.values())

    def _finish(self, tok, R, Wr):
        for t in R:
            tk = self._tk(t)
            tk.r[id(tok[0])] = tok
        for t in Wr:
            tk = self._tk(t)
            tk.w = tok
            tk.r = {}

    mute = False

    def I(self, eng, fn, R=(), W=()):
        if self.mute:
            return None
        e = self.E[eng]
        if e.n >= 60000:
            e.sem = self.new_sem(e.name)
            e.n = 0
        toks = self._collect(e, R, W)
        for tok in toks[1:]:
            e.h.wait_ge(tok[0], tok[1])
            e.mark(tok)
            self.ninst += 1
        ins = fn(e.h)
        if toks:
            ins._wait_ge(toks[0][0], toks[0][1])
            e.mark(toks[0])
        e.n += 1
        ins.then_inc(e.sem, 1)
        self.ninst += 1
        self._finish((e.sem, e.n), R, W)
        return ins

    def dma(self, q, out, in_, R=(), W=()):
        if self.mute:
            return None
        e = self.E[q]
        ds = self.dsq[q][self.dnext[q]]
        self.dnext[q] = (self.dnext[q] + 1) % len(self.dsq[q])
        toks = self._collect(e, R, W)
        if ds[1] > 0 and e.need((ds[0], ds[1])):
            toks.append((ds[0], ds[1]))
        for tok in toks[1:]:
            e.h.wait_ge(tok[0], tok[1])
            e.mark(tok)
            self.ninst += 1
        ins = e.h.dma_start(out=out, in_=in_)
        if toks:
            ins._wait_ge(toks[0][0], toks[0][1])
            e.mark(toks[0])
        if ds[1] >= 60000:
            ds[0] = self.new_sem("d")
            ds[1] = 0
        ds[1] += 16
        ins.then_inc(ds[0], 16)
        self.ninst += 1
        self._finish((ds[0], ds[1]), R, W)
        return ins

    def final_wait(self, toks):
        e = self.E["sp"]
        for tok in toks:
            if tok is not None and e.need(tok):
                e.h.wait_ge(tok[0], tok[1])
                e.mark(tok)


import os
SKIP = set(os.environ.get('KSKIP', '').split(','))


def build(S, L, dbg=()):
    NT = S // 128
    NBK = S // 512
    TOPK = min(256, S // 4)
    nc = bass.Bass("TRN2", target_bir_lowering=False)
    kb = KB(nc)

    def din(name, shape, dt=F32):
        return nc.dram_tensor(name, list(shape), dt, kind="ExternalInput").ap()

    def dscr(name, shape, dt=F32):
        return nc.dram_tensor(name, list(shape), dt, kind="Internal").ap()

    x_in = din("x", [S, D])
    pos_in = din("pos", [128, NT], I32)
    wD = din("wD", [L, D, N_D])
    wA = din("wA", [L, D, WA_COLS])
    wB = din("wB", [L, D, N_B])
    mu_in = din("mu_in", [L, N_B])
    lora = din("lora", [L, 160, W])
    rowv = din("rowv", [L, NROW, W])
    out_d = nc.dram_tensor("out", [S, D], F32, kind="ExternalOutput").ap()
    pv_in = din("pv", [128, L, NPV])
    pvf_in = din("pvf", [128, NCH])
    wG = din("wG", [L, D, N_G])
    wC = din("wC", [L, D, N_C])
    pool_w = din("pool_w", [L, 4, 128, 128])
    w_branch = din("w_branch", [L, NB, W, D])
    w_out = din("w_out", [L, D, D])
    mlp_up = din("mlp_up", [L, D, 4 * D])
    mlp_down = din("mlp_down", [L, 4 * D, D])

    wG_b = dscr("wG_b", [L, D, N_G], BF16)
    wC_b = dscr("wC_b", [L, D, N_C], BF16)
    wbr_b = dscr("wbr_b", [L, NB, W, D], BF16)
    wout_b = dscr("wout_b", [L, D, D], BF16)
    wup_b = dscr("wup_b", [L, D, 4 * D], BF16)
    wdn_b = dscr("wdn_b", [L, 4 * D, D], BF16)
    poolw_b = dscr("poolw_b", [L, 4, 128, 128], BF16)
    wD_b = dscr("wD_b", [L, D, N_D], BF16)
    wA_b = dscr("wA_b", [L, D, WA_COLS], BF16)
    wB_b = dscr("wB_b", [L, 2 * D, N_B], BF16)
    aT_d = dscr("aT_d", [64, 15, S], BF16)
    vaug_d = dscr("vaug_d", [128, NT, 130], BF16)
    wi_d = dscr("wi_d", [128, NT, 4], F32)

    xT = dscr("xT", [128, NCH, S])
    gatesT = dscr("gatesT", [128, 32, S], BF16)
    oT = [dscr("o%dT" % i, [128, 4, S], BF16) for i in range(4)]

    dbg_out = {}
    for name in dbg:
        if name == "xT":
            dbg_out[name] = nc.dram_tensor("dbg_xT", [128, NCH, S], F32, kind="ExternalOutput").ap()
        elif name.startswith("o") and name.endswith("T"):
            dbg_out[name] = nc.dram_tensor("dbg_" + name, [128, 4, S], BF16, kind="ExternalOutput").ap()
        elif name == "gatesT":
            dbg_out[name] = nc.dram_tensor("dbg_gatesT", [128, 32, S], BF16, kind="ExternalOutput").ap()

    I = kb.I
    dma = kb.dma

    psA = [kb.ps("psA%d" % i, [128, 512]) for i in range(3)]
    ps_i = [0]

    def pbank():
        t = psA[ps_i[0] % len(psA)]
        ps_i[0] += 1
        return t

    ps_n = kb.ps("ps_n", [128, 512])
    ps_t = kb.ps("ps_t", [128, 1024], BF16)
    ps_f = kb.ps("ps_f", [128, 512])


    ident_f = kb.sb("ident_f", [128, 128], F32)
    ident_b = kb.sb("ident_b", [128, 128], BF16)
    ones_b = kb.sb("ones_b", [128, 128], BF16)
    I("pool", lambda h: h.memset(ident_f[:], 1.0), W=[ident_f])
    I("pool", lambda h: h.affine_select(out=ident_f[:], in_=ident_f[:], pattern=[[-1, 128]], compare_op=ALU.is_equal,
                                        fill=0.0, base=0, channel_multiplier=1), R=[ident_f], W=[ident_f])
    I("pool", lambda h: h.tensor_copy(out=ident_b[:], in_=ident_f[:]), R=[ident_f], W=[ident_b])
    I("pool", lambda h: h.memset(ones_b[:], 1.0), W=[ones_b])
    eps_t = kb.sb("eps_t", [128, 1], F32)
    I("pool", lambda h: h.memset(eps_t[:], NORM_EPS), W=[eps_t])

    pv = kb.sb("pv_sb", [128, L, NPV], F32)
    g_fin = kb.sb("g_fin", [128, NCH], F32)
    dma("sp", pv[:], pv_in, W=[pv])
    dma("sp", g_fin[:], pvf_in, W=[g_fin])
    g_attn = g_mlp = gb_sb = pscale = pv

    pm_cur = kb.sb("pm_cur", [128, 4, 128], BF16)
    pm_prev = kb.sb("pm_prev", [128, 4, 128], BF16)
    pm_first = kb.sb("pm_first", [128, 4, 128], BF16)
    pm_tmp = kb.sb("pm_tmp", [128, 128], F32)
    pm_tmp2 = kb.sb("pm_tmp2", [128, 128], F32)
    for gi, win in enumerate(POOL_WINDOWS):
        I("pool", lambda h: h.memset(pm_tmp[:], 1.0 / win), W=[pm_tmp])
        I("pool", lambda h: h.affine_select(out=pm_tmp[:], in_=pm_tmp[:], pattern=[[1, 128]], compare_op=ALU.is_ge,
                                            fill=0.0, base=0, channel_multiplier=-1), R=[pm_tmp], W=[pm_tmp])
        I("pool", lambda h: h.affine_select(out=pm_tmp[:], in_=pm_tmp[:], pattern=[[-1, 128]], compare_op=ALU.is_ge,
                                            fill=0.0, base=win - 1, channel_multiplier=1), R=[pm_tmp], W=[pm_tmp])
        I("pool", lambda h: h.tensor_tensor(out=pm_tmp2[:], in0=pm_tmp[:], in1=ident_f[:], op=ALU.subtract),
          R=[pm_tmp, ident_f], W=[pm_tmp2])
        I("pool", lambda h: h.tensor_copy(out=pm_cur[:, gi, :], in_=pm_tmp2[:]), R=[pm_tmp2], W=[pm_cur])
        for t in range(win - 1):
            I("pool", lambda h: h.memset(pm_tmp[0:t + 1, t:t + 1], 1.0 / (t + 1)), R=[pm_tmp], W=[pm_tmp])
        I("pool", lambda h: h.tensor_tensor(out=pm_tmp2[:], in0=pm_tmp[:], in1=ident_f[:], op=ALU.subtract),
          R=[pm_tmp, ident_f], W=[pm_tmp2])
        I("pool", lambda h: h.tensor_copy(out=pm_first[:, gi, :], in_=pm_tmp2[:]), R=[pm_tmp2], W=[pm_first])
        I("pool", lambda h: h.memset(pm_tmp[:], 1.0 / win), W=[pm_tmp])
        I("pool", lambda h: h.affine_select(out=pm_tmp[:], in_=pm_tmp[:], pattern=[[-1, 128]], compare_op=ALU.is_ge,
                                            fill=0.0, base=win - 129, channel_multiplier=1), R=[pm_tmp], W=[pm_tmp])
        I("pool", lambda h: h.tensor_copy(out=pm_prev[:, gi, :], in_=pm_tmp[:]), R=[pm_tmp], W=[pm_prev])

    NANG = 48
    cs_d = dscr("cs_d", [128, NT, NANG])
    with kb.scope():
      if "cs" not in SKIP:
        cs_tab = kb.sb("cs_tab", [128, NT, NANG], F32)
        pos_i = kb.sb("pos_i", [128, NT], I32)
        posf = kb.sb("posf", [128, NT], F32)
        ang = kb.sb("ang", [128, NT, NANG], F32)
        ang_i = kb.sb("ang_i", [128, NT, NANG], I32)
        ang_k = kb.sb("ang_k", [128, NT, NANG], F32)
        ang_r = kb.sb("ang_r", [128, NT, NANG], F32)
        dma("sp", pos_i[:], pos_in, W=[pos_i])
        I("dve", lambda h: h.tensor_copy(out=posf[:], in_=pos_i[:]), R=[pos_i], W=[posf])
        fa = [500000.0 ** (-(2.0 * i) / 16.0) for i in range(8)]
        fr = [1.0 / (10000.0 ** (i / 15.0)) for i in range(16)]
        cols = [(f, math.pi / 2) for f in fa] + [(f, 0.0) for f in fa] + [(f, math.pi / 2) for f in fr] + [(f, 0.0) for f in fr]
        for ci, (f, ph) in enumerate(cols):
            I("dve", lambda h: h.tensor_scalar(out=ang[:, :, ci], in0=posf[:], scalar1=float(np.float32(f)), scalar2=ph,
                                               op0=ALU.mult, op1=ALU.add), R=[posf], W=[ang])
        I("dve", lambda h: h.tensor_scalar(out=ang_i[:], in0=ang[:], scalar1=1.0 / (2 * math.pi), scalar2=None, op0=ALU.mult),
          R=[ang], W=[ang_i])
        I("dve", lambda h: h.tensor_copy(out=ang_k[:], in_=ang_i[:]), R=[ang_i], W=[ang_k])
        I("dve", lambda h: h.scalar_tensor_tensor(out=ang_r[:], in0=ang_k[:], scalar=-2 * math.pi, in1=ang[:], op0=ALU.mult, op1=ALU.add),
          R=[ang_k, ang], W=[ang_r])
        I("dve", lambda h: h.tensor_scalar(out=ang_k[:], in0=ang_r[:], scalar1=math.pi, scalar2=-2 * math.pi, op0=ALU.is_gt, op1=ALU.mult),
          R=[ang_r], W=[ang_k])
        I("dve", lambda h: h.tensor_tensor(out=ang[:], in0=ang_r[:], in1=ang_k[:], op=ALU.add), R=[ang_r, ang_k], W=[ang])
        I("dve", lambda h: h.tensor_scalar(out=ang_k[:], in0=ang[:], scalar1=-math.pi, scalar2=2 * math.pi, op0=ALU.is_lt, op1=ALU.mult),
          R=[ang], W=[ang_k])
        I("dve", lambda h: h.tensor_tensor(out=ang_r[:], in0=ang[:], in1=ang_k[:], op=ALU.add), R=[ang, ang_k], W=[ang_r])
        I("act", lambda h: h.activation(out=cs_tab[:], in_=ang_r[:], func=AF.Sin), R=[ang_r], W=[cs_tab])
        dma("sp", cs_d, cs_tab[:], R=[cs_tab], W=[kb.dk(("cs", 0))])

    RS = 32.0 ** -0.5
    lg = [math.log(1.0 - 2.0 ** (-5.0 - hh)) for hh in range(8)]
    r_intra = kb.sb("r_intra", [128, 8, 128], F32)
    r_qd = kb.sb("r_qd", [128, 4, 128], F32)
    r_kdec = kb.sb("r_kdec", [128, 8], F32)
    r_cdec = kb.sb("r_cdec", [128, 4], F32)
    bdmask = kb.sb("bdmask", [128, 4, 128], F32)
    I("pool", lambda h: h.memset(bdmask[:], 0.0), W=[bdmask])
    I("pool", lambda h: h.memset(bdmask[0:64, :, 0:64], 1.0), W=[bdmask])
    I("pool", lambda h: h.memset(bdmask[64:128, :, 64:128], 1.0), W=[bdmask])
    with kb.scope():
        kb.mute = "rconst" in SKIP
        dmat = kb.sb("dmat", [128, 128], F32)
        nmat = kb.sb("nmat", [128, 128], F32)
        mcol = kb.sb("mcol", [128, 1], F32)
        bias_t = kb.sb("bias_t", [128, 24], F32)
        ustr = kb.sb("ustr", [128, 128], BF16)
        I("pool", lambda h: h.memset(ustr[:], 1.0), W=[ustr])
        I("pool", lambda h: h.affine_select(out=ustr[:], in_=ustr[:], pattern=[[1, 128]], compare_op=ALU.is_gt,
                                            fill=0.0, base=0, channel_multiplier=-1), R=[ustr], W=[ustr])
        I("pe", lambda h: h.matmul(ps_f[:, 0:128], lhsT=ones_b[:], rhs=ustr[:], start=True, stop=True), R=[ones_b, ustr], W=[ps_f])
        I("pe", lambda h: h.matmul(ps_f[:, 128:129], lhsT=ustr[:], rhs=ones_b[:, 0:1], start=True, stop=True), R=[ones_b, ustr], W=[ps_f])
        I("dve", lambda h: h.tensor_copy(out=nmat[:], in_=ps_f[:, 0:128]), R=[ps_f], W=[nmat])
        I("dve", lambda h: h.tensor_copy(out=mcol[:], in_=ps_f[:, 128:129]), R=[ps_f], W=[mcol])
        I("dve", lambda h: h.tensor_scalar(out=dmat[:], in0=nmat[:], scalar1=mcol[:, 0:1], scalar2=None, op0=ALU.subtract),
          R=[nmat, mcol], W=[dmat])
        for hh in range(8):
            I("pool", lambda h: h.memset(bias_t[:, hh:hh + 1], math.log(RS)), W=[bias_t])
            I("pool", lambda h: h.memset(bias_t[:, 8 + hh:9 + hh], lg[hh] + math.log(RS)), W=[bias_t])
            I("pool", lambda h: h.memset(bias_t[:, 16 + hh:17 + hh], 127.0 * lg[hh]), W=[bias_t])
        for hh in range(8):
            I("act", lambda h: h.activation(out=r_intra[:, hh, :], in_=dmat[:], func=AF.Exp, scale=lg[hh], bias=bias_t[:, hh:hh + 1]),
              R=[dmat, bias_t], W=[r_intra])
            c, hf = hh // 2, hh % 2
            I("act", lambda h: h.activation(out=r_qd[hf * 64:(hf + 1) * 64, c, :], in_=nmat[hf * 64:(hf + 1) * 64, :], func=AF.Exp,
                                            scale=lg[hh], bias=bias_t[hf * 64:(hf + 1) * 64, 8 + hh:9 + hh]), R=[nmat, bias_t], W=[r_qd])
            I("act", lambda h: h.activation(out=r_kdec[:, hh:hh + 1], in_=mcol[:], func=AF.Exp, scale=-lg[hh],
                                            bias=bias_t[:, 16 + hh:17 + hh]), R=[mcol, bias_t], W=[r_kdec])
            I("pool", lambda h: h.memset(r_cdec[hf * 64:(hf + 1) * 64, c:c + 1], math.exp(128.0 * lg[hh])), W=[r_cdec])
        I("pool", lambda h: h.affine_select(out=r_intra[:], in_=r_intra[:], pattern=[[0, 8], [1, 128]], compare_op=ALU.is_ge,
                                            fill=0.0, base=0, channel_multiplier=-1), R=[r_intra], W=[r_intra])

    kb.mute = False
    def cast_w(dst, src, key, nsplit):
        n = src.shape[0]
        step = n // nsplit
        for i in range(nsplit):
            dma("pool", dst[i * step:(i + 1) * step], src[i * step:(i + 1) * step], W=[kb.dk(key)])

    for l in range(L):
        cast_w(wG_b[l], wG[l], ("wG", l), 4)
        cast_w(wC_b[l], wC[l], ("wC", l), 1)
        cast_w(wD_b[l], wD[l], ("wD", l), 2)
        cast_w(wA_b[l], wA[l], ("wA", l), 2)
        cast_w(wbr_b[l].rearrange("i k n -> (i k) n"), w_branch[l].rearrange("i k n -> (i k) n"), ("wbr", l), 2)
        cast_w(wout_b[l], w_out[l], ("wout", l), 1)
        cast_w(wup_b[l], mlp_up[l], ("wup", l), 4)
        cast_w(wdn_b[l], mlp_down[l], ("wdn", l), 4)
        cast_w(poolw_b[l].rearrange("g c d -> (g c) d"), pool_w[l].rearrange("g c d -> (g c) d"), ("poolw", l), 1)

    with kb.scope():
        mu_bc = kb.sb("mu_bc", [128, N_B], F32)
        omu_bc = kb.sb("omu_bc", [128, N_B], F32)
        wst = [kb.sb("wst%d" % i, [128, N_B], F32) for i in range(2)]
        wlo = [kb.sb("wlo%d" % i, [128, N_B], BF16) for i in range(2)]
        whi = [kb.sb("whi%d" % i, [128, N_B], BF16) for i in range(2)]
        for l in range(L):
            dma("sp", mu_bc[:], mu_in[l, :].partition_broadcast(128), W=[mu_bc])
            I("dve", lambda h: h.tensor_scalar(out=omu_bc[:], in0=mu_bc[:], scalar1=-1.0, scalar2=1.0, op0=ALU.mult, op1=ALU.add),
              R=[mu_bc], W=[omu_bc])
            for c in range(NCH):
                ws, wl, wh = wst[c % 2], wlo[c % 2], whi[c % 2]
                dma("sp", ws[:], wB[l, c * 128:(c + 1) * 128, :], W=[ws])
                I("dve", lambda h: h.tensor_tensor(out=wl[:], in0=ws[:], in1=omu_bc[:], op=ALU.mult), R=[ws, omu_bc], W=[wl])
                I("pool", lambda h: h.tensor_tensor(out=wh[:], in0=ws[:], in1=mu_bc[:], op=ALU.mult), R=[ws, mu_bc], W=[wh])
                dma("sp", wB_b[l, c * 128:(c + 1) * 128, :], wl[:], R=[wl], W=[kb.dk(("wB", l))])
                dma("sp", wB_b[l, D + c * 128:D + (c + 1) * 128, :], wh[:], R=[wh], W=[kb.dk(("wB", l))])

    xin = kb.sb("xin", [128, NCH, 512], F32)
    sq = kb.sb("sq", [128, NCH, 512], BF16)
    rstd = kb.sb("rstd", [128, 512], F32)
    hTb = kb.sb("hT", [128, NCH, 520], BF16)
    hT = HView(hTb)
    NWT = 4
    wts = []

    def alloc_wts():
        wts[:] = [kb.sb("wt%d" % i, [128, 4096], BF16) for i in range(NWT)]
    wt_i = [0]

    def wtile():
        t = wts[wt_i[0] % NWT]
        wt_i[0] += 1
        return t

    def norm_block(g_tile, gsel):
        I("act", lambda h: h.activation(out=sq[:].rearrange("p c t -> p (c t)"), in_=xin[:].rearrange("p c t -> p (c t)"),
                                        func=AF.Square), R=[xin], W=[sq])
        for c in range(NCH):
            I("pe", lambda h: h.matmul(ps_n[:], lhsT=ones_b[:], rhs=sq[:, c, :], start=(c == 0), stop=(c == NCH - 1)),
              R=[ones_b, sq], W=[ps_n])
        I("act", lambda h: h.activation(out=rstd[:], in_=ps_n[:], func=AF.Sqrt, bias=eps_t[:, 0:1], scale=1.0 / D),
          R=[ps_n, eps_t], W=[rstd])
        I("dve", lambda h: h.reciprocal(out=rstd[:], in_=rstd[:]), R=[rstd], W=[rstd])
        for c in range(NCH):
            I("dve", lambda h: h.scalar_tensor_tensor(out=hT[:, c, :], in0=xin[:, c, :], scalar=gsel(c), in1=rstd[:],
                                                       op0=ALU.mult, op1=ALU.mult), R=[xin, rstd, g_tile], W=[hT])

    def group_norm(src_ps, o_sb, xc, sqt, stat, eps):
        I("act", lambda h: h.activation(out=o_sb[:], in_=src_ps[:], func=AF.Copy), R=[src_ps], W=[o_sb])
        o3_ = o_sb[:].rearrange("p (s d) -> p s d", d=64)
        x3_ = xc[:].rearrange("p (s d) -> p s d", d=64)
        s3_ = sqt[:].rearrange("p (s d) -> p s d", d=64)
        I("dve", lambda h: h.tensor_reduce(out=stat[:, 0:8], in_=o3_, axis=AX.X, op=ALU.add), R=[o_sb], W=[stat])
        I("dve", lambda h: h.tensor_scalar(out=stat[:, 8:16], in0=stat[:, 0:8], scalar1=1.0 / 64, scalar2=None, op0=ALU.mult), R=[stat], W=[stat])
        I("dve", lambda h: h.tensor_tensor(out=x3_, in0=o3_, in1=stat[:, 8:16].unsqueeze(2).to_broadcast([128, 8, 64]), op=ALU.subtract),
          R=[o_sb, stat], W=[xc])
        I("act", lambda h: h.activation(out=sqt[:], in_=xc[:], func=AF.Square), R=[xc], W=[sqt])
        I("dve", lambda h: h.tensor_reduce(out=stat[:, 16:24], in_=s3_, axis=AX.X, op=ALU.add), R=[sqt], W=[stat])
        I("dve", lambda h: h.tensor_scalar(out=stat[:, 16:24], in0=stat[:, 16:24], scalar1=1.0 / 64, scalar2=eps, op0=ALU.mult, op1=ALU.add),
          R=[stat], W=[stat])
        I("act", lambda h: h.activation(out=stat[:, 24:32], in_=stat[:, 16:24], func=AF.Sqrt), R=[stat], W=[stat])
        I("dve", lambda h: h.reciprocal(out=stat[:, 24:32], in_=stat[:, 24:32]), R=[stat], W=[stat])
        I("dve", lambda h: h.tensor_tensor(out=x3_, in0=x3_, in1=stat[:, 24:32].unsqueeze(2).to_broadcast([128, 8, 64]), op=ALU.mult),
          R=[xc, stat], W=[xc])

    with kb.scope():
        xtok = kb.sb("xtok", [128, D], F32)
        for t in range(NT):
            blk = t // 4
            sub = t % 4
            dma("sp", xtok[:], x_in[t * 128:(t + 1) * 128, :], W=[xtok])
            for half in range(2):
                for c4 in range(4):
                    c = half * 4 + c4
                    I("pe", lambda h: h.transpose(out=ps_f[:, c4 * 128:(c4 + 1) * 128], in_=xtok[:, c * 128:(c + 1) * 128],
                                                  identity=ident_f[:]), R=[xtok, ident_f], W=[ps_f])
                I("act" if half == 0 else "dve",
                  lambda h: (h.activation(out=xin[:, half * 4:(half + 1) * 4, sub * 128:(sub + 1) * 128],
                                          in_=ps_f[:].rearrange("p (c t) -> p c t", c=4), func=AF.Copy) if half == 0 else
                             h.tensor_copy(out=xin[:, half * 4:(half + 1) * 4, sub * 128:(sub + 1) * 128],
                                           in_=ps_f[:].rearrange("p (c t) -> p c t", c=4))),
                  R=[ps_f], W=[xin])
            if sub == 3:
                dma("sp", xT[:, :, blk * 512:(blk + 1) * 512], xin[:], R=[xin], W=[kb.dk(("xT", blk))])

    def phaseA(l):
        if "pha" in SKIP:
            return
        alloc_wts()
        pc_prev = kb.sb("pc_prev", [128, 512], BF16)
        pc_cur = [kb.sb("pc_cur%d" % i, [128, 512], BF16) for i in range(2)]
        pooledT = kb.sb("pooledT", [128, 4, 128], BF16)
        ocT_blk = kb.sb("ocT_blk", [128, 4, 512], BF16)
        gts = [kb.sb("gts%d" % i, [128, 512], BF16) for i in range(2)]
        zero_blk = kb.sb("zero_blk", [128, 4, 512], BF16)
        I("pool", lambda h: h.memset(zero_blk[:], 0.0), W=[zero_blk])
        poolw_sb = kb.sb("poolw_sb", [128, 4, 128], BF16)
        wC_sb = kb.sb("wC_sb", [128, NCH, 512], BF16)
        wD_sb = kb.sb("wD_sb", [128, NCH, N_D], BF16)
        wA_sb = kb.sb("wA_sb", [128, NCH, WA_COLS], BF16)
        cs_blk = kb.sb("cs_blk", [128, 4, 48], F32)
        dma("sp", wA_sb[:], wA_b[l].rearrange("(c p) n -> p c n", p=128), R=[kb.dk(("wA", l))], W=[wA_sb])
        arot_b = kb.sb("arot_b", [128, 960], BF16)
        at1 = kb.sb("at1", [128, 128], F32)
        at2 = kb.sb("at2", [128, 128], F32)
        aT_blk = kb.sb("aT_blk", [64, 15, 512], BF16)
        vaug_blk = kb.sb("vaug_blk", [128, 4, 130], BF16)
        wi_blk = kb.sb("wi_blk", [128, 4, 4], F32)
        I("pool", lambda h: h.memset(vaug_blk[:], 1.0), W=[vaug_blk])
        gng_bc = kb.sb("gng_bc", [128, W], F32)
        rqk_z = kb.sb("rqk_z", [128, 1024], BF16)
        kdk_z = kb.sb("kdk_z", [128, 512], BF16)
        I("pool", lambda h: h.memset(rqk_z[:], 0.0), W=[rqk_z])
        I("pool", lambda h: h.memset(kdk_z[:], 0.0), W=[kdk_z])
        rv_b = kb.sb("rv_b", [128, 512], BF16)
        rsil = kb.sb("rsil", [128, 512], F32)
        rt1 = kb.sb("rt1", [128, 256], F32)
        rt2 = kb.sb("rt2", [128, 256], F32)
        rT_sb = kb.sb("rT_sb", [128, 8, 128], BF16)
        qd_b = kb.sb("qd_b", [128, 4, 128], BF16)
        scm_b = kb.sb("scm_b", [128, 8, 128], BF16)
        rstate = kb.sb("rstate", [128, 4, 128], F32)
        rstate_b = kb.sb("rstate_b", [128, 4, 128], BF16)
        qbd = kb.sb("qbd", [128, 4, 256], BF16)
        I("pool", lambda h: h.memset(qbd[:], 0.0), W=[qbd])
        ro_sb = kb.sb("ro_sb", [128, 512], F32)
        ro_xc = kb.sb("ro_xc", [128, 512], F32)
        ro_sq = kb.sb("ro_sq", [128, 512], F32)
        gstat = kb.sb("gstat", [128, 32], F32)
        od_tm = kb.sb("od_tm", [128, 512], BF16)
        odT_blk = kb.sb("odT_blk", [128, 4, 512], BF16)
        dma("sp", wD_sb[:], wD_b[l].rearrange("(c p) n -> p c n", p=128), R=[kb.dk(("wD", l))], W=[wD_sb])
        dma("sp", gng_bc[:], rowv[l, RV_GNG, :].partition_broadcast(128), W=[gng_bc])
        I("pool", lambda h: h.memset(rstate[:], 0.0), W=[rstate])
        I("pool", lambda h: h.memset(rstate_b[:], 0.0), W=[rstate_b])
        dma("sp", wC_sb[:], wC_b[l].rearrange("(c p) n -> p c n", p=128), R=[kb.dk(("wC", l))], W=[wC_sb])
        dma("sp", poolw_sb[:], poolw_b[l].rearrange("g c d -> c g d"), R=[kb.dk(("poolw", l))], W=[poolw_sb])
        for blk in range(NBK):
            dma("sp", xin[:], xT[:, :, blk * 512:(blk + 1) * 512], R=[kb.dk(("xT", blk))], W=[xin])
            norm_block(g_attn, lambda c: pv[:, l, PV_AG + c:PV_AG + c + 1])
            dma("sp", cs_blk[:], cs_d[:, blk * 4:(blk + 1) * 4, :], R=[kb.dk(("cs", 0))], W=[cs_blk])
            for gq in range(8):
                wt = wtile()
                dma("sp", wt[:].rearrange("p (c n) -> p c n", c=NCH),
                    wG_b[l, :, gq * 512:(gq + 1) * 512].rearrange("(c p) n -> p c n", p=128),
                    R=[kb.dk(("wG", l))], W=[wt])
                wv = wt[:].rearrange("p (c n) -> p c n", c=NCH)
                for j in range(4):
                    ch = gq * 4 + j
                    pb = pbank()
                    for c in range(NCH):
                        I("pe", lambda h: h.matmul(pb[:], lhsT=wv[:, c, j * 128:(j + 1) * 128], rhs=hT[:, c, :],
                                                   start=(c == 0), stop=(c == NCH - 1)), R=[wt, hT], W=[pb])
                    gt = gts[ch % 2]
                    I("act", lambda h: h.activation(out=gt[:], in_=pb[:], func=AF.Sigmoid, bias=pv[:, l, PV_GB + ch:PV_GB + ch + 1]),
                      R=[pb, gb_sb], W=[gt])
                    dma("sp", gatesT[:, ch, blk * 512:(blk + 1) * 512], gt[:], R=[gt], W=[kb.dk(("gatesT", blk))])
            for sub in range(4):
                t = blk * 4 + sub
                tsl = slice(sub * 128, (sub + 1) * 128)
                kb.mute = "apro" in SKIP
                pa0 = pbank()
                pa1 = pbank()
                pa2 = pbank()
                for bi, (pbk, c0, c1) in enumerate(((pa0, 0, 512), (pa1, 512, 1024), (pa2, 1024, WA_COLS))):
                    for c in range(NCH):
                        I("pe", lambda h: h.matmul(pbk[:, 0:c1 - c0], lhsT=hT[:, c, tsl], rhs=wA_sb[:, c, c0:c1],
                                                   start=(c == 0), stop=(c == NCH - 1)), R=[hT, wA_sb], W=[pbk])
                kb.mute = kb.mute or ("apc" in SKIP)
                I("act", lambda h: h.activation(out=arot_b[:, 0:512], in_=pa0[:], func=AF.Copy), R=[pa0], W=[arot_b])
                I("act", lambda h: h.activation(out=arot_b[:, 512:960], in_=pa1[:, 0:448], func=AF.Copy), R=[pa1], W=[arot_b])
                kb.mute = kb.mute or ("ap0" in SKIP)
                for (pbk, ns, so) in ((pa0, 8, 0), (pa1, 7, 8)):
                    p3 = pbk[:, 0:ns * 64].rearrange("p (s d) -> p s d", d=64)
                    x1, x2 = p3[:, :, 0:8], p3[:, :, 8:16]
                    cosb = cs_blk[:, sub, 0:8].unsqueeze(1).to_broadcast([128, ns, 8])
                    sinb = cs_blk[:, sub, 8:16].unsqueeze(1).to_broadcast([128, ns, 8])
                    o3a = arot_b[:].rearrange("p (s d) -> p s d", d=64)[:, so:so + ns, :]
                    b1 = at1[:, 0:ns * 8].rearrange("p (s d) -> p s d", d=8)
                    b2 = at2[:, 0:ns * 8].rearrange("p (s d) -> p s d", d=8)
                    I("dve", lambda h: h.tensor_tensor(out=b1, in0=x1, in1=cosb, op=ALU.mult), R=[pbk, cs_tab, arot_b], W=[at1])
                    I("dve", lambda h: h.tensor_tensor(out=b2, in0=x2, in1=sinb, op=ALU.mult), R=[pbk, cs_blk], W=[at2])
                    I("dve", lambda h: h.tensor_tensor(out=o3a[:, :, 0:8], in0=b1, in1=b2, op=ALU.subtract), R=[at1, at2], W=[arot_b])
                    I("dve", lambda h: h.tensor_tensor(out=b1, in0=x2, in1=cosb, op=ALU.mult), R=[pbk, cs_blk], W=[at1])
                    I("dve", lambda h: h.tensor_tensor(out=b2, in0=x1, in1=sinb, op=ALU.mult), R=[pbk, cs_blk], W=[at2])
                    I("dve", lambda h: h.tensor_tensor(out=o3a[:, :, 8:16], in0=b1, in1=b2, op=ALU.add), R=[at1, at2], W=[arot_b])
                kb.mute = kb.mute or ("ap1" in SKIP)
                kb.mute = kb.mute or ("ap1" in SKIP)
                I("act", lambda h: h.activation(out=vaug_blk[:, sub, :].rearrange("p (g d) -> p g d", g=2)[:, :, 0:64],
                                                in_=pa2[:, 0:128].rearrange("p (g d) -> p g d", g=2), func=AF.Copy), R=[pa2], W=[vaug_blk])
                I("act", lambda h: h.activation(out=wi_blk[:, sub, :], in_=pa2[:, 128:132], func=AF.Copy), R=[pa2], W=[wi_blk])
                kb.mute = kb.mute or ("ap2" in SKIP)
                for s0 in (0, 8):
                    ns = min(8, 15 - s0)
                    for si in range(ns):
                        I("pe", lambda h: h.transpose(out=ps_t[0:64, si * 128:(si + 1) * 128], in_=arot_b[:, (s0 + si) * 64:(s0 + si + 1) * 64],
                                                      identity=ident_b[:]), R=[arot_b, ident_b], W=[ps_t])
                    I("act", lambda h: h.activation(out=aT_blk[:, s0:s0 + ns, tsl], in_=ps_t[0:64, 0:ns * 128].rearrange("p (s t) -> p s t", t=128),
                                                    func=AF.Copy), R=[ps_t], W=[aT_blk])
                kb.mute = False
                pb = pbank()
                for c in range(NCH):
                    I("pe", lambda h: h.matmul(pb[:], lhsT=hT[:, c, tsl], rhs=wC_sb[:, c, :], start=(c == 0), stop=(c == NCH - 1)),
                      R=[hT, wC_sb], W=[pb])
                pcc = pc_cur[t % 2]
                pcp = pc_cur[(t + 1) % 2]
                I("act", lambda h: h.activation(out=pcc[:], in_=pb[:], func=AF.Copy), R=[pb], W=[pcc])
                pb2 = pbank()
                for gi in range(4):
                    mc = pm_first if t == 0 else pm_cur
                    I("pe", lambda h: h.matmul(pb2[:, gi * 128:(gi + 1) * 128], lhsT=pcc[:, gi * 128:(gi + 1) * 128], rhs=mc[:, gi, :],
                                               start=True, stop=(t == 0)), R=[pcc, mc], W=[pb2])
                    if t > 0:
                        I("pe", lambda h: h.matmul(pb2[:, gi * 128:(gi + 1) * 128], lhsT=pcp[:, gi * 128:(gi + 1) * 128], rhs=pm_prev[:, gi, :],
                                                   start=False, stop=True), R=[pcp, pm_prev], W=[pb2])
                I("dve", lambda h: h.tensor_copy(out=pooledT[:].rearrange("p g t -> p (g t)"), in_=pb2[:]), R=[pb2], W=[pooledT])
                pb3 = pbank()
                for gi in range(4):
                    I("pe", lambda h: h.matmul(pb3[:, gi * 128:(gi + 1) * 128], lhsT=poolw_sb[:, gi, :], rhs=pooledT[:, gi, :],
                                               start=True, stop=True), R=[poolw_sb, pooledT], W=[pb3])
                for gi in range(4):
                    I("act", lambda h: h.activation(out=ocT_blk[:, gi, tsl], in_=pb3[:, gi * 128:(gi + 1) * 128], func=AF.Copy,
                                                    scale=pv[:, l, PV_PS + gi:PV_PS + gi + 1]), R=[pb3, pscale], W=[ocT_blk])
                kb.mute = "ret" in SKIP
                pq = pbank()
                pv_ = pbank()
                pg = pbank()
                for bi, pbk in enumerate((pq, pv_, pg)):
                    for c in range(NCH):
                        I("pe", lambda h: h.matmul(pbk[:], lhsT=hT[:, c, tsl], rhs=wD_sb[:, c, bi * 512:(bi + 1) * 512],
                                                   start=(c == 0), stop=(c == NCH - 1)), R=[hT, wD_sb], W=[pbk])
                pq3 = pq[:].rearrange("p (s d) -> p s d", d=32)
                x1, x2 = pq3[:, :, 0:16], pq3[:, :, 16:32]
                cosb = cs_blk[:, sub, 16:32].unsqueeze(1).to_broadcast([128, 16, 16])
                sinb = cs_blk[:, sub, 32:48].unsqueeze(1).to_broadcast([128, 16, 16])
                o3 = rqk_z[:].rearrange("p (s d) -> p s d", d=64)
                a1 = rt1[:].rearrange("p (s d) -> p s d", d=16)
                a2 = rt2[:].rearrange("p (s d) -> p s d", d=16)
                _m = kb.mute
                kb.mute = _m or ("r_rot" in SKIP)
                I("dve", lambda h: h.tensor_tensor(out=a1, in0=x1, in1=cosb, op=ALU.mult), R=[pq, cs_blk], W=[rt1])
                I("dve", lambda h: h.tensor_tensor(out=a2, in0=x2, in1=sinb, op=ALU.mult), R=[pq, cs_blk], W=[rt2])
                I("dve", lambda h: h.tensor_tensor(out=o3[:, :, 0:16], in0=a1, in1=a2, op=ALU.subtract), R=[rt1, rt2], W=[rqk_z])
                I("dve", lambda h: h.tensor_tensor(out=a1, in0=x2, in1=cosb, op=ALU.mult), R=[pq, cs_blk], W=[rt1])
                I("dve", lambda h: h.tensor_tensor(out=a2, in0=x1, in1=sinb, op=ALU.mult), R=[pq, cs_blk], W=[rt2])
                I("dve", lambda h: h.tensor_tensor(out=o3[:, :, 16:32], in0=a1, in1=a2, op=ALU.add), R=[rt1, rt2], W=[rqk_z])
                kb.mute = _m or ("r_cp" in SKIP)
                I("act", lambda h: h.activation(out=rv_b[:], in_=pv_[:], func=AF.Copy), R=[pv_], W=[rv_b])
                kb.mute = _m or ("r_sil" in SKIP)
                I("act", lambda h: h.activation(out=rsil[:], in_=pg[:], func=AF.Silu), R=[pg], W=[rsil])
                kb.mute = _m or ("r_kdk" in SKIP)
                I("dve", lambda h: h.tensor_tensor(out=kdk_z[:].rearrange("p (s d) -> p s d", d=64),
                                                   in0=o3[:, 8:16, :],
                                                   in1=r_kdec[:].unsqueeze(2).to_broadcast([128, 8, 64]), op=ALU.mult),
                  R=[rqk_z, r_kdec], W=[kdk_z])
                kb.mute = _m or ("ret2" in SKIP)
                for c8 in range(8):
                    I("pe", lambda h: h.transpose(out=ps_t[:, c8 * 128:(c8 + 1) * 128], in_=rqk_z[:, c8 * 128:(c8 + 1) * 128],
                                                  identity=ident_b[:]), R=[rqk_z, ident_b], W=[ps_t])
                I("act", lambda h: h.activation(out=rT_sb[:].rearrange("p c t -> p (c t)"), in_=ps_t[:], func=AF.Copy),
                  R=[ps_t], W=[rT_sb])
                I("dve", lambda h: h.tensor_tensor(out=qd_b[:], in0=rT_sb[:, 0:4, :], in1=r_qd[:], op=ALU.mult), R=[rT_sb, r_qd], W=[qd_b])
                kb.mute = kb.mute or ("ret4" in SKIP)
                I("dve", lambda h: h.tensor_copy(out=qbd[0:64, :, 0:128], in_=rT_sb[0:64, 0:4, :]), R=[rT_sb], W=[qbd])
                I("dve", lambda h: h.tensor_copy(out=qbd[64:128, :, 128:256], in_=rT_sb[64:128, 0:4, :]), R=[rT_sb], W=[qbd])
                psc = [pbank(), pbank()]
                for c in range(4):
                    I("pe", lambda h: h.matmul(psc[c // 2][:, (c % 2) * 256:(c % 2 + 1) * 256], lhsT=rT_sb[:, 4 + c, :], rhs=qbd[:, c, :],
                                               start=True, stop=True), R=[rT_sb, qbd], W=[psc[c // 2]])
                for c2 in range(2):
                    I("dve", lambda h: h.tensor_tensor(out=scm_b[:, c2 * 4:(c2 + 1) * 4, :], in0=psc[c2][:].rearrange("p (s n) -> p s n", n=128),
                                                       in1=r_intra[:, c2 * 4:(c2 + 1) * 4, :], op=ALU.mult), R=[psc[c2], r_intra], W=[scm_b])
                kb.mute = kb.mute or ("r5" in SKIP)
                po = pbank()
                for c in range(4):
                    I("pe", lambda h: h.matmul(po[:, c * 128:(c + 1) * 128], lhsT=qd_b[:, c, :], rhs=rstate_b[:, c, :],
                                               start=True, stop=False), R=[qd_b, rstate_b], W=[po])
                    for j in range(2):
                        hh = 2 * c + j
                        I("pe", lambda h: h.matmul(po[:, hh * 64:(hh + 1) * 64], lhsT=scm_b[:, hh, :], rhs=rv_b[:, hh * 64:(hh + 1) * 64],
                                                   start=False, stop=(j == 1)), R=[scm_b, rv_b], W=[po])
                kb.mute = kb.mute or ("r6" in SKIP)
                pst = pbank()
                for c in range(4):
                    I("pe", lambda h: h.matmul(pst[:, c * 128:(c + 1) * 128], lhsT=kdk_z[:, c * 128:(c + 1) * 128],
                                               rhs=rv_b[:, c * 128:(c + 1) * 128], start=True, stop=True), R=[kdk_z, rv_b], W=[pst])
                kb.mute = kb.mute or ("r7" in SKIP)
                I("dve", lambda h: h.tensor_tensor(out=ro_sq[:], in0=pst[:], in1=bdmask[:].rearrange("p c d -> p (c d)"), op=ALU.mult),
                  R=[pst, bdmask], W=[ro_sq])
                I("dve", lambda h: h.tensor_tensor(out=rstate[:], in0=rstate[:], in1=r_cdec[:].unsqueeze(2).to_broadcast([128, 4, 128]), op=ALU.mult),
                  R=[rstate, r_cdec], W=[rstate])
                I("dve", lambda h: h.tensor_tensor(out=rstate[:].rearrange("p c d -> p (c d)"), in0=rstate[:].rearrange("p c d -> p (c d)"),
                                                   in1=ro_sq[:], op=ALU.add), R=[rstate, ro_sq], W=[rstate])
                I("dve", lambda h: h.tensor_copy(out=rstate_b[:], in_=rstate[:]), R=[rstate], W=[rstate_b])
                kb.mute = kb.mute or ("ret3" in SKIP)
                group_norm(po, ro_sb, ro_xc, ro_sq, gstat, RET_GN_EPS)
                I("dve", lambda h: h.tensor_tensor(out=ro_xc[:], in0=ro_xc[:], in1=gng_bc[:], op=ALU.mult), R=[ro_xc, gng_bc], W=[ro_xc])
                I("dve", lambda h: h.tensor_tensor(out=od_tm[:], in0=ro_xc[:], in1=rsil[:], op=ALU.mult), R=[ro_xc, rsil], W=[od_tm])
                for c4 in range(4):
                    I("pe", lambda h: h.transpose(out=ps_t[:, c4 * 128:(c4 + 1) * 128], in_=od_tm[:, c4 * 128:(c4 + 1) * 128],
                                                  identity=ident_b[:]), R=[od_tm, ident_b], W=[ps_t])
                I("act", lambda h: h.activation(out=odT_blk[:, :, tsl], in_=ps_t[:, 0:512].rearrange("p (c t) -> p c t", c=4), func=AF.Copy),
                  R=[ps_t], W=[odT_blk])
            kb.mute = False
            dma("sp", oT[2][:, :, blk * 512:(blk + 1) * 512], ocT_blk[:], R=[ocT_blk], W=[kb.dk(("o2T", blk))])
            dma("sp", oT[3][:, :, blk * 512:(blk + 1) * 512], odT_blk[:], R=[odT_blk], W=[kb.dk(("o3T", blk))])
            dma("sp", aT_d[:, :, blk * 512:(blk + 1) * 512], aT_blk[:], R=[aT_blk], W=[kb.dk(("aT", blk))])
            dma("sp", vaug_d[:, blk * 4:(blk + 1) * 4, :], vaug_blk[:], R=[vaug_blk], W=[kb.dk(("vaug", blk))])
            dma("sp", wi_d[:, blk * 4:(blk + 1) * 4, :], wi_blk[:], R=[wi_blk], W=[kb.dk(("wi", blk))])

    def phaseB(l):
        if "rwkv" in SKIP:
            return
        CD = math.exp(-0.5)
        psX = [kb.ps("psX0", [128, 512]), kb.ps("psX1", [128, 512])]
        psA.extend(psX)
        f32t = lambda n: kb.sb(n, [128, 512], F32)
        b16t = lambda n: kb.sb(n, [128, 512], BF16)
        wB_sb = kb.sb("wB_sb", [128, 16, N_B], BF16)
        dma("sp", wB_sb[:], wB_b[l].rearrange("(c p) n -> p c n", p=128), R=[kb.dk(("wB", l))], W=[wB_sb])
        bc = {}
        for nm, ri in (("w0", RV_W0), ("a0", RV_A0), ("kk", RV_KK), ("ka", RV_KA), ("rk", RV_RK), ("lng", RV_LNG), ("lnb", RV_LNB)):
            bc[nm] = f32t("bc_" + nm)
            dma("sp", bc[nm][:], rowv[l, ri, :].partition_broadcast(128), W=[bc[nm]])
        lora_b = kb.sb("lora_b", [96, 3, W], BF16)
        with kb.scope():
            lora_f = kb.sb("lora_f", [96, 3, W], F32)
            I("pool", lambda h: h.memset(lora_f[:], 0.0), W=[lora_f])
            dma("sp", lora_f[0:32, 0, :], lora[l, 0:32, :], W=[lora_f])
            dma("sp", lora_f[0:32, 1, :], lora[l, 32:64, :], W=[lora_f])
            dma("sp", lora_f[0:96, 2, :], lora[l, 64:160, :], W=[lora_f])
            I("dve", lambda h: h.tensor_copy(out=lora_b[:], in_=lora_f[:]), R=[lora_f], W=[lora_b])
        Lm = kb.sb("Lm", [128, 3, 128], F32)
        negc = kb.sb("negc", [128, 1], F32)
        msk = kb.sb("msk", [128, 3, 128], BF16)
        I("pool", lambda h: h.memset(Lm[:], -CD), W=[Lm])
        I("pool", lambda h: h.memset(negc[:], -CD), W=[negc])
        I("pool", lambda h: h.memset(msk[:], 1.0), W=[msk])
        for (tile_, j, pat, cm, op) in ((Lm, 0, 1, -1, ALU.is_ge), (Lm, 1, 1, -1, ALU.is_gt), (Lm, 2, -1, 1, ALU.is_gt),
                                        (msk, 0, 1, -1, ALU.is_gt), (msk, 1, 1, -1, ALU.is_ge), (msk, 2, -1, 1, ALU.is_gt)):
            I("pool", lambda h: h.affine_select(out=tile_[:, j, :], in_=tile_[:, j, :], pattern=[[pat, 128]], compare_op=op,
                                                fill=0.0, base=0, channel_multiplier=cm), R=[tile_], W=[tile_])
        r_f, k_f, v_f, sgw, a_f, g_f, kkf, kpf, b_f, tA, tB = [f32t(n) for n in ("r_f", "k_f", "v_f", "sgw", "a_f", "g_f", "kkf", "kpf", "b_f", "tA", "tB")]
        Ep, En, Eex, Erev = [f32t(n) for n in ("Ep", "En", "Eex", "Erev")]
        al_tm, be_tm, ka_tm, rh_tm, bp_tm, kp_tm, v_b, U_b, ob_tm = [b16t(n) for n in
            ("al_tm", "be_tm", "ka_tm", "rh_tm", "bp_tm", "kp_tm", "v_b", "U_b", "ob_tm")]
        XT = kb.sb("XT", [128, 4, 4, 128], BF16)
        Xbd = kb.sb("Xbd", [128, 3, 4, 256], BF16)
        I("pool", lambda h: h.memset(Xbd[:], 0.0), W=[Xbd])
        M5 = kb.sb("M5", [128, 5, 8, 128], BF16)
        CHD = F32
        chA = [kb.sb("chA%d" % i, [128, 4, 128], CHD) for i in range(2)]
        chAT = [kb.sb("chAT%d" % i, [128, 4, 128], CHD) for i in range(2)]
        chP = [kb.sb("chP%d" % i, [128, 4, 128], CHD) for i in range(2)]
        MT = kb.sb("MT", [128, 8, 128], CHD)
        RHS_c = kb.sb("RHS_c", [128, 512], CHD)
        ST_f = kb.sb("ST_f", [128, 4, 128], F32)
        ST_b = kb.sb("ST_b", [128, 4, 128], BF16)
        I("pool", lambda h: h.memset(ST_f[:], 0.0), W=[ST_f])
        I("pool", lambda h: h.memset(ST_b[:], 0.0), W=[ST_b])
        st8 = kb.sb("st8", [128, 40], F32)
        dC = kb.sb("dC", [128, 4], F32)
        gstat = kb.sb("gstatb", [128, 32], F32)
        lw = kb.sb("lw", [96, 3, 512], BF16)
        obT_blk = kb.sb("obT_blk", [128, 4, 512], BF16)
        I("pool", lambda h: h.memset(hTb[:], 0.0), W=[hTb])
        for blk in range(NBK):
            dma("sp", xin[:], xT[:, :, blk * 512:(blk + 1) * 512], R=[kb.dk(("xT", blk))], W=[xin])
            if blk > 0:
                I("dve", lambda h: h.tensor_copy(out=hTb[:, :, 7:8], in_=hTb[:, :, 519:520]), R=[hTb], W=[hTb])
            I("act", lambda h: h.activation(out=sq[:].rearrange("p c t -> p (c t)"), in_=xin[:].rearrange("p c t -> p (c t)"),
                                            func=AF.Square), R=[xin], W=[sq])
            for c in range(NCH):
                I("pe", lambda h: h.matmul(ps_n[:], lhsT=ones_b[:], rhs=sq[:, c, :], start=(c == 0), stop=(c == NCH - 1)),
                  R=[ones_b, sq], W=[ps_n])
            I("act", lambda h: h.activation(out=rstd[:], in_=ps_n[:], func=AF.Sqrt, bias=eps_t[:, 0:1], scale=1.0 / D),
              R=[ps_n, eps_t], W=[rstd])
            I("dve", lambda h: h.reciprocal(out=rstd[:], in_=rstd[:]), R=[rstd], W=[rstd])
            for c in range(NCH):
                I("dve", lambda h: h.scalar_tensor_tensor(out=hTb[:, c, 8:520], in0=xin[:, c, :], scalar=pv[:, l, PV_AG + c:PV_AG + c + 1],
                                                           in1=rstd[:], op0=ALU.mult, op1=ALU.mult), R=[xin, rstd, pv], W=[hTb])
            for gi_, (c0, cn, fn_) in enumerate(((1536, 32, AF.Tanh), (1568, 32, AF.Copy), (1600, 96, AF.Sigmoid))):
                pb = pbank()
                for c in range(16):
                    off = 8 if c < 8 else 7
                    I("pe", lambda h: h.matmul(pb[0:cn, :], lhsT=wB_sb[:, c, c0:c0 + cn], rhs=hTb[:, c % 8, off:off + 512],
                                               start=(c == 0), stop=(c == 15)), R=[wB_sb, hTb], W=[pb])
                I("act", lambda h: h.activation(out=lw[0:cn, gi_, :], in_=pb[0:cn, :], func=fn_), R=[pb], W=[lw])
            for sub in range(4):
                t = blk * 4 + sub
                tsl = slice(sub * 128, (sub + 1) * 128)
                for gi_, dst in enumerate((r_f, k_f, v_f)):
                    pb = pbank()
                    for c in range(16):
                        off = (8 if c < 8 else 7) + sub * 128
                        I("pe", lambda h: h.matmul(pb[:], lhsT=hTb[:, c % 8, off:off + 128], rhs=wB_sb[:, c, gi_ * 512:(gi_ + 1) * 512],
                                                   start=(c == 0), stop=(c == 15)), R=[wB_sb, hTb], W=[pb])
                    I("act", lambda h: h.activation(out=dst[:], in_=pb[:], func=AF.Copy), R=[pb], W=[dst])
                I("act", lambda h: h.activation(out=v_b[:], in_=v_f[:], func=AF.Copy), R=[v_f], W=[v_b])
                pzw = pbank()
                I("pe", lambda h: h.matmul(pzw[:], lhsT=lw[0:32, 0, tsl], rhs=lora_b[0:32, 0, :], start=True, stop=True), R=[lw, lora_b], W=[pzw])
                I("dve", lambda h: h.tensor_tensor(out=tA[:], in0=pzw[:], in1=bc["w0"][:], op=ALU.add), R=[pzw, bc["w0"]], W=[tA])
                I("act", lambda h: h.activation(out=sgw[:], in_=tA[:], func=AF.Sigmoid), R=[tA], W=[sgw])
                pza = pbank()
                I("pe", lambda h: h.matmul(pza[:], lhsT=lw[0:32, 1, tsl], rhs=lora_b[0:32, 1, :], start=True, stop=True), R=[lw, lora_b], W=[pza])
                I("dve", lambda h: h.tensor_tensor(out=tB[:], in0=pza[:], in1=bc["a0"][:], op=ALU.add), R=[pza, bc["a0"]], W=[tB])
                I("act", lambda h: h.activation(out=a_f[:], in_=tB[:], func=AF.Sigmoid), R=[tB], W=[a_f])
                pg_ = pbank()
                I("pe", lambda h: h.matmul(pg_[:], lhsT=lw[0:96, 2, tsl], rhs=lora_b[0:96, 2, :], start=True, stop=True), R=[lw, lora_b], W=[pg_])
                I("act", lambda h: h.activation(out=g_f[:], in_=pg_[:], func=AF.Copy), R=[pg_], W=[g_f])
                v3 = lambda tl: tl[:].rearrange("p (s d) -> p s d", d=64)
                I("pool", lambda h: h.tensor_tensor(out=kkf[:], in0=k_f[:], in1=bc["kk"][:], op=ALU.mult), R=[k_f, bc["kk"]], W=[kkf])
                I("act", lambda h: h.activation(out=tA[:], in_=kkf[:], func=AF.Square), R=[kkf], W=[tA])
                I("dve", lambda h: h.tensor_reduce(out=st8[:, 0:8], in_=v3(tA), axis=AX.X, op=ALU.add), R=[tA], W=[st8])
                I("act", lambda h: h.activation(out=st8[:, 8:16], in_=st8[:, 0:8], func=AF.Sqrt), R=[st8], W=[st8])
                I("dve", lambda h: h.tensor_scalar(out=st8[:, 8:16], in0=st8[:, 8:16], scalar1=1e-12, scalar2=None, op0=ALU.max), R=[st8], W=[st8])
                I("dve", lambda h: h.reciprocal(out=st8[:, 16:24], in_=st8[:, 8:16]), R=[st8], W=[st8])
                I("dve", lambda h: h.tensor_tensor(out=v3(kkf), in0=v3(kkf), in1=st8[:, 16:24].unsqueeze(2).to_broadcast([128, 8, 64]), op=ALU.mult),
                  R=[kkf, st8], W=[kkf])
                I("dve", lambda h: h.scalar_tensor_tensor(out=tB[:], in0=a_f[:], scalar=-1.0, in1=bc["ka"][:], op0=ALU.add, op1=ALU.mult),
                  R=[a_f, bc["ka"]], W=[tB])
                I("dve", lambda h: h.scalar_tensor_tensor(out=kpf[:], in0=tB[:], scalar=1.0, in1=k_f[:], op0=ALU.add, op1=ALU.mult),
                  R=[tB, k_f], W=[kpf])
                I("dve", lambda h: h.tensor_tensor(out=b_f[:], in0=kkf[:], in1=a_f[:], op=ALU.mult), R=[kkf, a_f], W=[b_f])
                I("pool", lambda h: h.tensor_tensor(out=tA[:], in0=r_f[:], in1=kpf[:], op=ALU.mult), R=[r_f, kpf], W=[tA])
                I("pool", lambda h: h.tensor_tensor(out=tA[:], in0=tA[:], in1=bc["rk"][:], op=ALU.mult), R=[tA, bc["rk"]], W=[tA])
                I("dve", lambda h: h.tensor_reduce(out=st8[:, 24:32], in_=v3(tA), axis=AX.X, op=ALU.add), R=[tA], W=[st8])
                pcs = []
                for j in range(3):
                    pb = pbank()
                    I("pe", lambda h: h.matmul(pb[:], lhsT=Lm[:, j, :], rhs=sgw[:], start=True, stop=True), R=[Lm, sgw], W=[pb])
                    pcs.append(pb)
                I("act", lambda h: h.activation(out=Ep[:], in_=pcs[0][:], func=AF.Exp), R=[pcs[0]], W=[Ep])
                I("act", lambda h: h.activation(out=En[:], in_=pcs[0][:], func=AF.Exp, scale=-1.0), R=[pcs[0]], W=[En])
                I("act", lambda h: h.activation(out=Eex[:], in_=pcs[1][:], func=AF.Exp), R=[pcs[1]], W=[Eex])
                I("act", lambda h: h.activation(out=Erev[:], in_=pcs[2][:], func=AF.Exp), R=[pcs[2]], W=[Erev])
                for p_ in range(4):
                    I("pe", lambda h: h.matmul(ps_f[:, p_:p_ + 1], lhsT=sgw[:, p_ * 128:(p_ + 1) * 128], rhs=negc[:], start=True, stop=True),
                      R=[sgw, negc], W=[ps_f])
                I("act", lambda h: h.activation(out=dC[:], in_=ps_f[:, 0:4], func=AF.Exp), R=[ps_f], W=[dC])
                I("dve", lambda h: h.tensor_tensor(out=al_tm[:], in0=kkf[:], in1=Eex[:], op=ALU.mult), R=[kkf, Eex], W=[al_tm])
                I("pool", lambda h: h.tensor_tensor(out=be_tm[:], in0=b_f[:], in1=En[:], op=ALU.mult), R=[b_f, En], W=[be_tm])
                I("dve", lambda h: h.tensor_tensor(out=ka_tm[:], in0=kpf[:], in1=En[:], op=ALU.mult), R=[kpf, En], W=[ka_tm])
                I("pool", lambda h: h.tensor_tensor(out=rh_tm[:], in0=r_f[:], in1=Ep[:], op=ALU.mult), R=[r_f, Ep], W=[rh_tm])
                I("dve", lambda h: h.tensor_tensor(out=bp_tm[:], in0=b_f[:], in1=Erev[:], op=ALU.mult), R=[b_f, Erev], W=[bp_tm])
                I("pool", lambda h: h.tensor_tensor(out=kp_tm[:], in0=kpf[:], in1=Erev[:], op=ALU.mult), R=[kpf, Erev], W=[kp_tm])
                for half, (ta, tb_) in enumerate(((al_tm, be_tm), (ka_tm, rh_tm))):
                    for xi, src in enumerate((ta, tb_)):
                        for p_ in range(4):
                            I("pe", lambda h: h.transpose(out=ps_t[:, (xi * 4 + p_) * 128:(xi * 4 + p_ + 1) * 128], in_=src[:, p_ * 128:(p_ + 1) * 128],
                                                          identity=ident_b[:]), R=[src, ident_b], W=[ps_t])
                    I("act", lambda h: h.activation(out=XT[:, half * 2:half * 2 + 2, :, :].rearrange("p x q t -> p (x q t)"), in_=ps_t[:], func=AF.Copy),
                      R=[ps_t], W=[XT])
                for bi_, xi in enumerate((0, 1, 3)):
                    I("dve", lambda h: h.tensor_copy(out=Xbd[0:64, bi_, :, 0:128], in_=XT[0:64, xi, :, :]), R=[XT], W=[Xbd])
                    I("pool", lambda h: h.tensor_copy(out=Xbd[64:128, bi_, :, 128:256], in_=XT[64:128, xi, :, :]), R=[XT], W=[Xbd])
                specs = ((1, 0, 0), (0, 1, 2), (2, 0, 0), (1, 2, 1), (2, 2, 1))
                for ti, (lt, rb, mk) in enumerate(specs):
                    for rnd in range(2):
                        pb = pbank()
                        for pp in range(2):
                            p_ = rnd * 2 + pp
                            I("pe", lambda h: h.matmul(pb[:, pp * 256:(pp + 1) * 256], lhsT=XT[:, lt, p_, :], rhs=Xbd[:, rb, p_, :],
                                                       start=True, stop=True), R=[XT, Xbd], W=[pb])
                        I("dve", lambda h: h.tensor_tensor(out=M5[:, ti, rnd * 4:(rnd + 1) * 4, :], in0=pb[:].rearrange("p (a t) -> p a t", t=128),
                                                           in1=msk[:, mk, :].unsqueeze(1).to_broadcast([128, 4, 128]), op=ALU.mult),
                          R=[pb, msk], W=[M5])
                for rnd in range(2):
                    hs = slice(rnd * 4, (rnd + 1) * 4)
                    A_, AT_ = M5[:, 1, hs, :], M5[:, 0, hs, :]
                    Abuf, ATbuf = M5, M5
                    I("dve", lambda h: h.tensor_tensor(out=chP[0][:], in0=ident_b[:].unsqueeze(1).to_broadcast([128, 4, 128]), in1=AT_, op=ALU.subtract),
                      R=[ident_b, M5], W=[chP[0]])
                    pcur = 0
                    for k_ in range(6):
                        pa_ = pbank()
                        for j in range(4):
                            I("pe", lambda h: h.matmul(pa_[:, j * 128:(j + 1) * 128], lhsT=AT_[:, j, :], rhs=A_[:, j, :], start=True, stop=True),
                              R=[Abuf, ATbuf], W=[pa_])
                        if k_ < 5:
                            pat_ = pbank()
                            for j in range(4):
                                I("pe", lambda h: h.matmul(pat_[:, j * 128:(j + 1) * 128], lhsT=A_[:, j, :], rhs=AT_[:, j, :], start=True, stop=True),
                                  R=[Abuf, ATbuf], W=[pat_])
                        nA = chA[k_ % 2]
                        I("act", lambda h: h.activation(out=nA[:].rearrange("p a t -> p (a t)"), in_=pa_[:], func=AF.Copy), R=[pa_], W=[nA])
                        if k_ < 5:
                            nAT = chAT[k_ % 2]
                            I("dve", lambda h: h.tensor_copy(out=nAT[:].rearrange("p a t -> p (a t)"), in_=pat_[:]), R=[pat_], W=[nAT])
                        pp_ = pbank()
                        for j in range(4):
                            I("pe", lambda h: h.matmul(pp_[:, j * 128:(j + 1) * 128], lhsT=nA[:, j, :], rhs=chP[pcur][:, j, :], start=True, stop=True),
                              R=[nA, chP[pcur]], W=[pp_])
                        dstP = MT[:, hs, :] if k_ == 5 else chP[1 - pcur][:]
                        dstB = MT if k_ == 5 else chP[1 - pcur]
                        I("dve", lambda h: h.tensor_tensor(out=dstP, in0=pp_[:].rearrange("p (a t) -> p a t", t=128), in1=chP[pcur][:], op=ALU.add),
                          R=[pp_, chP[pcur]], W=[dstB])
                        pcur = 1 - pcur
                        if k_ < 5:
                            A_, AT_ = nA[:], nAT[:]
                            Abuf, ATbuf = nA, nAT
                prhs = pbank()
                for p_ in range(4):
                    I("pe", lambda h: h.matmul(prhs[:, p_ * 128:(p_ + 1) * 128], lhsT=XT[:, 0, p_, :], rhs=ST_b[:, p_, :], start=True, stop=False),
                      R=[XT, ST_b], W=[prhs])
                    for j in range(2):
                        hd = p_ * 2 + j
                        I("pe", lambda h: h.matmul(prhs[:, hd * 64:(hd + 1) * 64], lhsT=M5[:, 2, hd, :], rhs=v_b[:, hd * 64:(hd + 1) * 64],
                                                   start=False, stop=(j == 1)), R=[M5, v_b], W=[prhs])
                I("act", lambda h: h.activation(out=RHS_c[:], in_=prhs[:], func=AF.Copy), R=[prhs], W=[RHS_c])
                pu = pbank()
                for hd in range(8):
                    I("pe", lambda h: h.matmul(pu[:, hd * 64:(hd + 1) * 64], lhsT=MT[:, hd, :], rhs=RHS_c[:, hd * 64:(hd + 1) * 64],
                                               start=True, stop=True), R=[MT, RHS_c], W=[pu])
                I("act", lambda h: h.activation(out=U_b[:], in_=pu[:], func=AF.Copy, scale=-1.0), R=[pu], W=[U_b])
                py = pbank()
                for p_ in range(4):
                    I("pe", lambda h: h.matmul(py[:, p_ * 128:(p_ + 1) * 128], lhsT=XT[:, 3, p_, :], rhs=ST_b[:, p_, :], start=True, stop=False),
                      R=[XT, ST_b], W=[py])
                    for j in range(2):
                        hd = p_ * 2 + j
                        I("pe", lambda h: h.matmul(py[:, hd * 64:(hd + 1) * 64], lhsT=M5[:, 3, hd, :], rhs=U_b[:, hd * 64:(hd + 1) * 64],
                                                   start=False, stop=False), R=[M5, U_b], W=[py])
                        I("pe", lambda h: h.matmul(py[:, hd * 64:(hd + 1) * 64], lhsT=M5[:, 4, hd, :], rhs=v_b[:, hd * 64:(hd + 1) * 64],
                                                   start=False, stop=(j == 1)), R=[M5, v_b], W=[py])
                pst = pbank()
                for p_ in range(4):
                    psl = slice(p_ * 128, (p_ + 1) * 128)
                    I("pe", lambda h: h.matmul(pst[:, psl], lhsT=bp_tm[:, psl], rhs=U_b[:, psl], start=True, stop=False), R=[bp_tm, U_b], W=[pst])
                    I("pe", lambda h: h.matmul(pst[:, psl], lhsT=kp_tm[:, psl], rhs=v_b[:, psl], start=False, stop=True), R=[kp_tm, v_b], W=[pst])
                I("dve", lambda h: h.tensor_tensor(out=tA[:], in0=pst[:], in1=bdmask[:].rearrange("p c d -> p (c d)"), op=ALU.mult),
                  R=[pst, bdmask], W=[tA])
                I("dve", lambda h: h.tensor_tensor(out=ST_f[:], in0=ST_f[:], in1=dC[:].unsqueeze(2).to_broadcast([128, 4, 128]), op=ALU.mult),
                  R=[ST_f, dC], W=[ST_f])
                I("dve", lambda h: h.tensor_tensor(out=ST_f[:].rearrange("p c d -> p (c d)"), in0=ST_f[:].rearrange("p c d -> p (c d)"), in1=tA[:], op=ALU.add),
                  R=[ST_f, tA], W=[ST_f])
                I("dve", lambda h: h.tensor_copy(out=ST_b[:], in_=ST_f[:]), R=[ST_f], W=[ST_b])
                group_norm(py, Ep, En, Eex, gstat, RWKV_GN_EPS)
                I("dve", lambda h: h.tensor_tensor(out=En[:], in0=En[:], in1=bc["lng"][:], op=ALU.mult), R=[En, bc["lng"]], W=[En])
                I("pool", lambda h: h.tensor_tensor(out=En[:], in0=En[:], in1=bc["lnb"][:], op=ALU.add), R=[En, bc["lnb"]], W=[En])
                I("dve", lambda h: h.tensor_tensor(out=v3(tB), in0=v3(v_f), in1=st8[:, 24:32].unsqueeze(2).to_broadcast([128, 8, 64]), op=ALU.mult),
                  R=[v_f, st8], W=[tB])
                I("pool", lambda h: h.tensor_tensor(out=En[:], in0=En[:], in1=tB[:], op=ALU.add), R=[En, tB], W=[En])
                I("dve", lambda h: h.tensor_tensor(out=ob_tm[:], in0=En[:], in1=g_f[:], op=ALU.mult), R=[En, g_f], W=[ob_tm])
                for c4 in range(4):
                    I("pe", lambda h: h.transpose(out=ps_t[:, c4 * 128:(c4 + 1) * 128], in_=ob_tm[:, c4 * 128:(c4 + 1) * 128],
                                                  identity=ident_b[:]), R=[ob_tm, ident_b], W=[ps_t])
                I("act", lambda h: h.activation(out=obT_blk[:, :, tsl], in_=ps_t[:, 0:512].rearrange("p (c t) -> p c t", c=4), func=AF.Copy),
                  R=[ps_t], W=[obT_blk])
            dma("sp", oT[1][:, :, blk * 512:(blk + 1) * 512], obT_blk[:], R=[obT_blk], W=[kb.dk(("o1T", blk))])
        del psA[3:]

    def phaseAttn(l):
        if "attn" in SKIP:
            return
        NIT = 14
        SCALE = 64.0 ** -0.5
        kT = kb.sb("kT", [64, 2, S], BF16)
        kiT = kb.sb("kiT", [64, S], BF16)
        vaug = kb.sb("vaug", [128, NT, 130], BF16)
        allk = [kb.dk(("aT", b)) for b in range(NBK)]
        dma("sp", kT[:], aT_d[:, 8:10, :], R=allk, W=[kT])
        dma("sp", kiT[:], aT_d[:, 14, :], R=allk, W=[kiT])
        dma("sp", vaug[:], vaug_d, R=[kb.dk(("vaug", b)) for b in range(NBK)], W=[vaug])
        idx = kb.sb("idx", [128, S], F32)
        maskb = kb.sb("maskb", [128, S], BF16)
        maskT = kb.sb("maskT", [128, NT, 128], BF16)
        qT = kb.sb("qT", [64, 8, 128], BF16)
        qiT = kb.sb("qiT", [64, 4, 128], BF16)
        wi_t = kb.sb("wi_t", [128, 4], F32)
        dg = kb.sb("dg", [128, 4, 128], BF16)
        relu4 = [kb.sb("relu4_%d" % i, [128, 512], BF16) for i in range(8)]
        ebuf = [kb.sb("ebuf%d" % i, [128, 512], BF16) for i in range(4)]
        embuf = [kb.sb("embuf%d" % i, [128, 512], BF16) for i in range(4)]
        bs = kb.sb("bs", [128, 16], F32)
        steps = kb.sb("steps", [128, 3 * NIT], F32)
        pw2 = kb.sb("pw2", [128, NIT], F32)
        osb = kb.sb("osb", [65, 512], F32)
        sel65 = kb.sb("sel65", [65, 64], F32)
        rden = kb.sb("rden", [64, 512], F32)
        onorm = kb.sb("onorm", [64, 512], BF16)
        pacc = [kb.ps("pacc0", [128, 512]), kb.ps("pacc1", [128, 512])]
        for k in range(NIT):
            I("pool", lambda h: h.memset(pw2[:, k:k + 1], 2.0 ** -(k + 1)), W=[pw2])
        cmask = kb.sb("cmask", [128, 128], F32)
        I("pool", lambda h: h.memset(cmask[:], 0.0), W=[cmask])
        I("pool", lambda h: h.affine_select(out=cmask[:], in_=cmask[:], pattern=[[-1, 128]], compare_op=ALU.is_ge,
                                            fill=-30000.0, base=0, channel_multiplier=1), R=[cmask], W=[cmask])
        I("pool", lambda h: h.memset(bs[:], 0.0), W=[bs])
        I("pool", lambda h: h.memset(bs[:, 10:11], TOPK - 0.5), W=[bs])
        I("pool", lambda h: h.memset(sel65[:], 0.0), W=[sel65])
        I("pool", lambda h: h.memset(sel65[64:65, :], 1.0), W=[sel65])
        for qt in range(NT):
            kb.mute = False
            nk = (qt + 1) * 128
            nkb = qt + 1
            qsl = slice(qt * 128, (qt + 1) * 128)
            dma("sp", qT[:], aT_d[:, 0:8, qsl], R=allk, W=[qT])
            dma("sp", qiT[:], aT_d[:, 10:14, qsl], R=allk, W=[qiT])
            dma("sp", wi_t[:], wi_d[:, qt, :], R=[kb.dk(("wi", qt // 4))], W=[wi_t])
            for hh in range(4):
                I("dve", lambda h: h.tensor_scalar(out=dg[:, hh, :], in0=ident_b[:], scalar1=wi_t[:, hh:hh + 1], scalar2=None, op0=ALU.mult),
                  R=[ident_b, wi_t], W=[dg])
            kb.mute = "at0" in SKIP
            nchunk = (nk + 511) // 512

            def idx_s1(kc):
                c0 = kc * 512
                ncol = min(512, nk - c0)
                rset = (kc % 2) * 4
                for hh in range(4):
                    pb = pbank()
                    I("pe", lambda h: h.matmul(pb[:, 0:ncol], lhsT=qiT[:, hh, :], rhs=kiT[:, c0:c0 + ncol], start=True, stop=True),
                      R=[qiT, kiT], W=[pb])
                    I("act", lambda h: h.activation(out=relu4[rset + hh][:, 0:ncol], in_=pb[:, 0:ncol], func=AF.Relu), R=[pb], W=[relu4[rset + hh]])

            def idx_d(kc):
                c0 = kc * 512
                ncol = min(512, nk - c0)
                rset = (kc % 2) * 4
                psum_h = ps_n if kc % 2 == 0 else ps_f
                for hh in range(4):
                    I("pe", lambda h: h.matmul(psum_h[:, 0:ncol], lhsT=dg[:, hh, :], rhs=relu4[rset + hh][:, 0:ncol], start=(hh == 0), stop=(hh == 3)),
                      R=[dg, relu4[rset + hh]], W=[psum_h])
                I("dve", lambda h: h.tensor_copy(out=idx[:, c0:c0 + ncol], in_=psum_h[:, 0:ncol]), R=[psum_h], W=[idx])

            idx_s1(0)
            for kc in range(nchunk):
                if kc + 1 < nchunk:
                    idx_s1(kc + 1)
                idx_d(kc)
            kb.mute = kb.mute or ("at1" in SKIP)
            I("dve", lambda h: h.tensor_reduce(out=bs[:, 0:1], in_=idx[:, 0:nk], axis=AX.X, op=ALU.min), R=[idx], W=[bs])
            I("pool", lambda h: h.tensor_tensor(out=idx[:, qsl], in0=idx[:, qsl], in1=cmask[:], op=ALU.add), R=[idx, bs, cmask], W=[idx])
            I("dve", lambda h: h.tensor_reduce(out=bs[:, 1:2], in_=idx[:, 0:nk], axis=AX.X, op=ALU.max), R=[idx], W=[bs])
            I("dve", lambda h: h.tensor_scalar(out=bs[:, 6:7], in0=bs[:, 0:1], scalar1=-1e-3, scalar2=None, op0=ALU.add), R=[bs], W=[bs])
            I("dve", lambda h: h.tensor_tensor(out=bs[:, 2:3], in0=bs[:, 1:2], in1=bs[:, 6:7], op=ALU.subtract), R=[bs], W=[bs])
            I("dve", lambda h: h.tensor_scalar(out=bs[:, 2:3], in0=bs[:, 2:3], scalar1=1e-3, scalar2=None, op0=ALU.add), R=[bs], W=[bs])
            I("dve", lambda h: h.tensor_scalar(out=steps[:, 0:NIT], in0=pw2[:], scalar1=bs[:, 2:3], scalar2=None, op0=ALU.mult), R=[pw2, bs], W=[steps])
            I("dve", lambda h: h.tensor_scalar(out=steps[:, NIT:2 * NIT], in0=steps[:, 0:NIT], scalar1=-1.0, scalar2=None, op0=ALU.mult), R=[steps], W=[steps])
            I("dve", lambda h: h.tensor_scalar(out=steps[:, 2 * NIT:3 * NIT], in0=steps[:, 0:NIT], scalar1=2.0, scalar2=None, op0=ALU.mult), R=[steps], W=[steps])
            mcol = 3
            I("dve", lambda h: h.tensor_tensor(out=bs[:, mcol:mcol + 1], in0=bs[:, 6:7], in1=steps[:, 0:1], op=ALU.add), R=[bs, steps], W=[bs])
            for k in range(NIT):
                I("dve", lambda h: h.tensor_scalar(out=maskb[:, 0:nk], in0=idx[:, 0:nk], scalar1=bs[:, mcol:mcol + 1], scalar2=bs[:, 8:9],
                                                   op0=ALU.is_ge, op1=ALU.add, accum_out=bs[:, 4:5]), R=[idx, bs], W=[maskb, bs])
                last = (k == NIT - 1)
                pcol = (NIT + k) if last else (2 * NIT + k + 1)
                ncolm = (NIT + k) if last else (NIT + k + 1)
                pcol = k if last else pcol
                I("dve", lambda h: h.tensor_scalar(out=bs[:, 5:6], in0=bs[:, 4:5], scalar1=bs[:, 10:11], scalar2=steps[:, pcol:pcol + 1],
                                                   op0=ALU.is_ge, op1=ALU.mult), R=[bs, steps], W=[bs])
                ncolx = 9 if mcol == 3 else 3
                I("dve", lambda h: h.scalar_tensor_tensor(out=bs[:, ncolx:ncolx + 1], in0=bs[:, 5:6], scalar=steps[:, ncolm:ncolm + 1],
                                                           in1=bs[:, mcol:mcol + 1], op0=ALU.add, op1=ALU.add), R=[bs, steps], W=[bs])
                mcol = ncolx
            lo = mcol
            I("dve", lambda h: h.tensor_scalar(out=maskb[:, 0:nk], in0=idx[:, 0:nk], scalar1=bs[:, lo:lo + 1], scalar2=None, op0=ALU.is_ge),
              R=[idx, bs], W=[maskb])
            for k0 in range(0, nkb, 8):
                nn = min(8, nkb - k0)
                for j in range(nn):
                    I("pe", lambda h: h.transpose(out=ps_t[:, j * 128:(j + 1) * 128], in_=maskb[:, (k0 + j) * 128:(k0 + j + 1) * 128],
                                                  identity=ident_b[:]), R=[maskb, ident_b], W=[ps_t])
                I("act", lambda h: h.activation(out=maskT[:, k0:k0 + nn, :].rearrange("p k t -> p (k t)"), in_=ps_t[:, 0:nn * 128], func=AF.Copy),
                  R=[ps_t], W=[maskT])
            kb.mute = kb.mute or ("at2" in SKIP)
            nst = 2 * nkb
            pbs = {}

            def pv_s(i):
                kbi, g = i // 2, i % 2
                pb = pbank()
                I("pe", lambda h: h.matmul(pb[:], lhsT=kT[:, g, kbi * 128:(kbi + 1) * 128], rhs=qT[:, g * 4:(g + 1) * 4, :], start=True, stop=True),
                  R=[kT, qT], W=[pb])
                pbs[i] = pb

            def pv_e(i):
                kbi, g = i // 2, i % 2
                pb = pbs.pop(i)
                eb, em = ebuf[i % 4], embuf[i % 4]
                I("act", lambda h: h.activation(out=eb[:], in_=pb[:], func=AF.Exp, scale=SCALE), R=[pb], W=[eb])
                I("dve", lambda h: h.tensor_tensor(out=em[:].rearrange("p (j t) -> p j t", j=4), in0=eb[:].rearrange("p (j t) -> p j t", j=4),
                                                   in1=maskT[:, kbi, :].unsqueeze(1).to_broadcast([128, 4, 128]), op=ALU.mult),
                  R=[eb, maskT], W=[em])

            def pv_m(i):
                kbi, g = i // 2, i % 2
                em = embuf[i % 4]
                I("pe", lambda h: h.matmul(pacc[g][0:65, :], lhsT=vaug[:, kbi, g * 65:(g + 1) * 65], rhs=em[:],
                                           start=(kbi == 0), stop=(kbi == nkb - 1)), R=[vaug, em], W=[pacc[g]])

            for i in range(nst + 2):
                if i < nst:
                    pv_s(i)
                if 0 <= i - 1 < nst:
                    pv_e(i - 1)
                if 0 <= i - 2 < nst:
                    pv_m(i - 2)
            kb.mute = kb.mute or ("at3" in SKIP)
            for g in range(2):
                I("act", lambda h: h.activation(out=osb[:], in_=pacc[g][0:65, :], func=AF.Copy), R=[pacc[g]], W=[osb])
                I("pe", lambda h: h.matmul(ps_f[0:64, :], lhsT=sel65[:], rhs=osb[:], start=True, stop=True), R=[sel65, osb], W=[ps_f])
                I("dve", lambda h: h.reciprocal(out=rden[:], in_=ps_f[0:64, :]), R=[ps_f], W=[rden])
                I("dve", lambda h: h.tensor_tensor(out=onorm[:], in0=osb[0:64, :], in1=rden[:], op=ALU.mult), R=[osb, rden], W=[onorm])
                for j in range(4):
                    hd = g * 4 + j
                    dma("sp", oT[0][(hd % 2) * 64:(hd % 2) * 64 + 64, hd // 2, qsl], onorm[:, j * 128:(j + 1) * 128],
                        R=[onorm], W=[kb.dk(("o0T", qt // 4))])

    def phaseC(l):
        kb.mute = False
        if "phc" in SKIP:
            return
        alloc_wts()
        oin = [kb.sb("oin%d" % i, [128, 4, 512], BF16) for i in range(4)]
        gin = kb.sb("gin", [128, 32, 512], BF16)
        merged = kb.sb("merged", [128, NCH, 512], F32)
        merged_b = kb.sb("merged_b", [128, NCH, 512], BF16)
        actT = kb.sb("actT", [128, 32, 512], BF16)
        relu_t = [kb.sb("relu%d" % i, [128, 512], BF16) for i in range(2)]
        otok = kb.sb("otok", [128, D], F32)
        hTf = kb.sb("hTf", [128, NCH, 512], F32)
        for blk in range(NBK):
            bsl = slice(blk * 512, (blk + 1) * 512)
            dma("sp", xin[:], xT[:, :, bsl], R=[kb.dk(("xT", blk))], W=[xin])
            for i in range(4):
                dma("sp", oin[i][:], oT[i][:, :, bsl], R=[kb.dk(("o%dT" % i, blk))], W=[oin[i]])
            for q4 in range(4):
                dma("sp", gin[:, q4 * 8:(q4 + 1) * 8, :], gatesT[:, q4 * 8:(q4 + 1) * 8, bsl], R=[kb.dk(("gatesT", blk))], W=[gin])
            for i in range(4):
                wt = wtile()
                wv = wt[:].rearrange("p (c n) -> p c n", c=4)
                dma("sp", wv, wbr_b[l, i].rearrange("(c p) n -> p c n", p=128), R=[kb.dk(("wbr", l))], W=[wt])
                for oc in range(NCH):
                    pb = pbank()
                    for c in range(4):
                        I("pe", lambda h: h.matmul(pb[:], lhsT=wv[:, c, oc * 128:(oc + 1) * 128], rhs=oin[i][:, c, :],
                                                   start=(c == 0), stop=(c == 3)), R=[wt, oin[i]], W=[pb])
                    if i == 0:
                        I("dve", lambda h: h.tensor_tensor(out=merged[:, oc, :], in0=pb[:], in1=gin[:, i * 8 + oc, :], op=ALU.mult),
                          R=[pb, gin], W=[merged])
                    else:
                        I("dve", lambda h: h.tensor_tensor(out=relu_t[0][:], in0=pb[:], in1=gin[:, i * 8 + oc, :], op=ALU.mult),
                          R=[pb, gin], W=[relu_t[0]])
                        if i < 3:
                            I("pool", lambda h: h.tensor_tensor(out=merged[:, oc, :], in0=merged[:, oc, :], in1=relu_t[0][:], op=ALU.add),
                              R=[merged, relu_t[0]], W=[merged])
                        else:
                            I("pool", lambda h: h.tensor_tensor(out=merged_b[:, oc, :], in0=merged[:, oc, :], in1=relu_t[0][:], op=ALU.add),
                              R=[merged, relu_t[0]], W=[merged_b])
            for half in range(2):
                wt = wtile()
                wv = wt[:].rearrange("p (c n) -> p c n", c=NCH)
                dma("sp", wv, wout_b[l, :, half * 512:(half + 1) * 512].rearrange("(c p) n -> p c n", p=128),
                    R=[kb.dk(("wout", l))], W=[wt])
                for j in range(4):
                    oc = half * 4 + j
                    pb = pbank()
                    for c in range(NCH):
                        I("pe", lambda h: h.matmul(pb[:], lhsT=wv[:, c, j * 128:(j + 1) * 128], rhs=merged_b[:, c, :],
                                                   start=(c == 0), stop=(c == NCH - 1)), R=[wt, merged_b], W=[pb])
                    I("dve", lambda h: h.tensor_tensor(out=xin[:, oc, :], in0=xin[:, oc, :], in1=pb[:], op=ALU.add),
                      R=[pb, xin], W=[xin])
            norm_block(g_mlp, lambda c: pv[:, l, PV_MG + c:PV_MG + c + 1])
            for f8 in range(8):
                wt = wtile()
                wv = wt[:].rearrange("p (c n) -> p c n", c=NCH)
                dma("sp", wv, wup_b[l, :, f8 * 512:(f8 + 1) * 512].rearrange("(c p) n -> p c n", p=128),
                    R=[kb.dk(("wup", l))], W=[wt])
                for j in range(4):
                    f = f8 * 4 + j
                    pb = pbank()
                    for c in range(NCH):
                        I("pe", lambda h: h.matmul(pb[:], lhsT=wv[:, c, j * 128:(j + 1) * 128], rhs=hT[:, c, :],
                                                   start=(c == 0), stop=(c == NCH - 1)), R=[wt, hT], W=[pb])
                    rt = relu_t[1]
                    I("act", lambda h: h.activation(out=rt[:], in_=pb[:], func=AF.Relu), R=[pb], W=[rt])
                    I("pool", lambda h: h.tensor_tensor(out=actT[:, f, :], in0=rt[:], in1=rt[:], op=ALU.mult), R=[rt], W=[actT])
            for oc in range(NCH):
                wt = wtile()
                wv = wt[:].rearrange("p (c n) -> p c n", c=32)
                dma("sp", wv, wdn_b[l, :, oc * 128:(oc + 1) * 128].rearrange("(c p) n -> p c n", p=128),
                    R=[kb.dk(("wdn", l))], W=[wt])
                pb = pbank()
                for c in range(32):
                    I("pe", lambda h: h.matmul(pb[:], lhsT=wv[:, c, :], rhs=actT[:, c, :], start=(c == 0), stop=(c == 31)),
                      R=[wt, actT], W=[pb])
                I("dve", lambda h: h.tensor_tensor(out=xin[:, oc, :], in0=xin[:, oc, :], in1=pb[:], op=ALU.add),
                  R=[pb, xin], W=[xin])
            if l < L - 1:
                dma("sp", xT[:, :, bsl], xin[:], R=[xin], W=[kb.dk(("xT", blk))])
            else:
                if "xT" in dbg_out:
                    dma("sp", dbg_out["xT"][:, :, bsl], xin[:], R=[xin], W=[kb.dk(("dbgx", blk))])
                I("act", lambda h: h.activation(out=sq[:].rearrange("p c t -> p (c t)"), in_=xin[:].rearrange("p c t -> p (c t)"),
                                                func=AF.Square), R=[xin], W=[sq])
                for c in range(NCH):
                    I("pe", lambda h: h.matmul(ps_n[:], lhsT=ones_b[:], rhs=sq[:, c, :], start=(c == 0), stop=(c == NCH - 1)),
                      R=[ones_b, sq], W=[ps_n])
                I("act", lambda h: h.activation(out=rstd[:], in_=ps_n[:], func=AF.Sqrt, bias=eps_t[:, 0:1], scale=1.0 / D),
                  R=[ps_n, eps_t], W=[rstd])
                I("dve", lambda h: h.reciprocal(out=rstd[:], in_=rstd[:]), R=[rstd], W=[rstd])
                for c in range(NCH):
                    I("dve", lambda h: h.scalar_tensor_tensor(out=hTf[:, c, :], in0=xin[:, c, :], scalar=g_fin[:, c:c + 1], in1=rstd[:],
                                                               op0=ALU.mult, op1=ALU.mult), R=[xin, rstd, g_fin], W=[hTf])
                for sub in range(4):
                    t = blk * 4 + sub
                    for half in range(2):
                        for c4 in range(4):
                            c = half * 4 + c4
                            I("pe", lambda h: h.transpose(out=ps_f[:, c4 * 128:(c4 + 1) * 128], in_=hTf[:, c, sub * 128:(sub + 1) * 128],
                                                          identity=ident_f[:]), R=[hTf, ident_f], W=[ps_f])
                        I("act", lambda h: h.activation(out=otok[:, half * 512:(half + 1) * 512], in_=ps_f[:], func=AF.Copy),
                          R=[ps_f], W=[otok])
                    dma("sp", out_d[t * 128:(t + 1) * 128, :], otok[:], R=[otok], W=[kb.dk(("out", t))])

    for l in range(L):
        with kb.scope():
            phaseA(l)
        with kb.scope():
            phaseB(l)
        with kb.scope():
            phaseAttn(l)
        with kb.scope():
            phaseC(l)

    for name, ap in dbg_out.items():
        if name == "xT":
            continue
        src = {"gatesT": gatesT, "o0T": oT[0], "o1T": oT[1], "o2T": oT[2], "o3T": oT[3]}[name]
        keys = [k for k in kb.dram_tk if k[0] == name]
        dma("sp", ap, src, R=[kb.dram_tk[k] for k in keys], W=[kb.dk(("dbg", name))])

    kb.final_wait([tk.w for key, tk in kb.dram_tk.items() if key[0] in ("out", "dbg", "dbgx")])
    kb.es.close()
    build.ninst = kb.ninst
    return nc


def prep_inputs(inputs, b, L):
    f = lambda a: np.ascontiguousarray(np.asarray(a), dtype=np.float32)
    w_in = np.asarray(inputs["w_in"])
    m = {
        "x": f(inputs["x"][b]),
        "pos": np.ascontiguousarray(np.asarray(inputs["positions"][b]).astype(np.int32).reshape(-1, 128).T),
        "wD": f(w_in[:, :, N_A + N_B + N_C:N_A + N_B + N_C + N_D]),
        "wB": f(np.concatenate([w_in[:, :, 1092:1604], w_in[:, :, 1636:2148], w_in[:, :, 2148:2660], w_in[:, :, 1604:1636],
                                w_in[:, :, 2660:2692], w_in[:, :, 2692:2788]], axis=2)),
        "mu_in": f(np.concatenate([np.asarray(inputs["rwkv_mu"])[:, i:j] for i, j in ((0, 512), (544, 1056), (1056, 1568), (512, 544), (1568, 1600), (1600, 1696))], axis=1)),
        "lora": f(np.concatenate([np.asarray(inputs["rwkv_w2"]), np.asarray(inputs["rwkv_a2"]), np.asarray(inputs["rwkv_g2"])], axis=1)),
        "wA": f(np.concatenate([w_in[:, :, 0:512], w_in[:, :, 512:640], w_in[:, :, 768:1088],
                                np.zeros_like(w_in[:, :, 0:64]), w_in[:, :, 640:768], w_in[:, :, 1088:1092]], axis=2)),
        "wG": f(w_in[:, :, N_A + N_B + N_C + N_D:]),
        "wC": f(w_in[:, :, N_A + N_B:N_A + N_B + N_C]),
        "pool_w": f(inputs["pool_w"]),
        "w_branch": f(inputs["w_branch"]),
        "w_out": f(inputs["w_out"]),
        "mlp_up": f(inputs["mlp_up"]),
        "mlp_down": f(inputs["mlp_down"]),
    }
    pv = np.zeros((128, L, NPV), np.float32)
    for l in range(L):
        pv[:, l, PV_AG:PV_AG + 8] = colmajor(inputs["attn_norm_g"][l])
        pv[:, l, PV_MG:PV_MG + 8] = colmajor(inputs["mlp_norm_g"][l])
        pv[:, l, PV_GB:PV_GB + 32] = colmajor(np.asarray(inputs["gate_b"][l]).reshape(-1))
        pv[:, l, PV_PS:PV_PS + 4] = colmajor(inputs["pool_scale"][l])
    m["pv"] = pv
    rowv = np.zeros((L, NROW, W), np.float32)
    rowv[:, RV_GNG] = np.asarray(inputs["ret_gn_g"])
    for ri, nm in ((RV_W0, "rwkv_w0"), (RV_A0, "rwkv_a0"), (RV_KK, "rwkv_k_k"), (RV_KA, "rwkv_k_a"), (RV_RK, "rwkv_r_k"),
                   (RV_LNG, "rwkv_ln_g"), (RV_LNB, "rwkv_ln_b")):
        rowv[:, ri] = np.asarray(inputs[nm])
    m["rowv"] = rowv
    m["pvf"] = colmajor(inputs["final_norm_g"])
    return m


def kernel(**inputs):
    x = np.asarray(inputs["x"])
    B, S, _ = x.shape
    L = np.asarray(inputs["w_in"]).shape[0]
    nc = build(S, L)
    in_maps = [prep_inputs(inputs, b, L) for b in range(B)]
    res = run_bass_kernel_spmd(nc, in_maps, core_ids=list(range(B)))
    return np.stack([np.asarray(r["out"]) for r in res.results], axis=0).astype(np.float32)
```

```python
import math
from contextlib import ExitStack

import numpy as np
import concourse.bass as bass
import concourse.mybir as mybir
from concourse.bass_utils import run_bass_kernel_spmd

F32 = mybir.dt.float32
BF16 = mybir.dt.bfloat16
I32 = mybir.dt.int32
ALU = mybir.AluOpType
AF = mybir.ActivationFunctionType
AX = mybir.AxisListType

D = 1024
NCH = 8
W = 512
NB = 4
NORM_EPS = 1e-5
N_A, N_B, N_C, N_D = 1092, 1696, 512, 1536
N_G = 4096
WA_COLS = 1156
POOL_WINDOWS = (2, 4, 8, 16)
PV_AG, PV_MG, PV_GB, PV_PS = 0, 8, 16, 48
NPV = 64
RV_GNG, RV_W0, RV_A0, RV_KK, RV_KA, RV_RK, RV_LNG, RV_LNB = range(8)
NROW = 12
RWKV_GN_EPS = 64e-5
RET_GN_EPS = 1e-6


def colmajor(v):
    v = np.asarray(v, dtype=np.float32)
    return np.ascontiguousarray(v.reshape(-1, 128).T)


class Tk:
    __slots__ = ("w", "r")

    def __init__(self):
        self.w = None
        self.r = {}


class Buf:
    def __init__(self, t):
        self.t = t
        self.k = Tk()

    def __getitem__(self, idx):
        return self.t[idx]


class HView(Buf):
    def __init__(self, buf):
        self.t = buf.t
        self.k = buf.k

    def __getitem__(self, idx):
        p, c, sl = idx
        a = 0 if sl.start is None else sl.start
        b = 512 if sl.stop is None else sl.stop
        return self.t[p, c, 8 + a:8 + b]


class Eng:
    def __init__(self, kb, name, handle, is_pe=False):
        self.kb = kb
        self.name = name
        self.h = handle
        self.is_pe = is_pe
        self.sem = kb.new_sem(name)
        self.n = 0
        self.seen = {}
        self.cnt = 0

    def need(self, tok):
        if tok is None:
            return False
        sem, val = tok
        if self.is_pe and sem is self.sem:
            return False
        return self.seen.get(id(sem), 0) < val

    def mark(self, tok):
        self.seen[id(tok[0])] = max(self.seen.get(id(tok[0]), 0), tok[1])


class KB:
    def __init__(self, nc):
        self.nc = nc
        self.es = ExitStack()
        self.stack = [self.es]
        self.nsem = 0
        self.E = {
            "pe": Eng(self, "pe", nc.tensor, is_pe=True),
            "dve": Eng(self, "dve", nc.vector),
            "act": Eng(self, "act", nc.scalar),
            "pool": Eng(self, "pool", nc.gpsimd),
            "sp": Eng(self, "sp", nc.sync),
        }
        self.dsq = {"sp": [[self.new_sem("dsp%d" % i), 0] for i in range(16)],
                    "pool": [[self.new_sem("dpl%d" % i), 0] for i in range(8)],
                    "act": [[self.new_sem("dac%d" % i), 0] for i in range(4)]}
        self.dsems = [x for v in self.dsq.values() for x in v]
        self.dnext = {"sp": 0, "pool": 0, "act": 0}
        self.ninst = 0
        self.dram_tk = {}

    def new_sem(self, name):
        self.nsem += 1
        return self.es.enter_context(self.nc.semaphore("s_%s_%d" % (name, self.nsem)))

    def sb(self, name, shape, dt):
        self.nname = getattr(self, "nname", 0) + 1
        return Buf(self.stack[-1].enter_context(self.nc.sbuf_tensor("%s_%d" % (name, self.nname), list(shape), dt)))

    def ps(self, name, shape, dt=F32):
        self.nname = getattr(self, "nname", 0) + 1
        b = Buf(self.stack[-1].enter_context(self.nc.psum_tensor("%s_%d" % (name, self.nname), list(shape), dt)))
        b.psum = True
        return b

    def barrier(self):
        toks = [(o.sem, o.n) for o in self.E.values() if o.n > 0]
        toks += [(ds[0], ds[1]) for ds in self.dsems if ds[1] > 0]
        for e in self.E.values():
            for tok in toks:
                if tok[0] is e.sem:
                    continue
                if e.need(tok):
                    e.h.wait_ge(tok[0], tok[1])
                    e.mark(tok)
                    self.ninst += 1

    def scope(self):
        kb = self

        class _Sc:
            def __enter__(self_):
                self_.es = ExitStack()
                kb.stack.append(self_.es)

            def __exit__(self_, *a):
                kb.barrier()
                kb.stack.pop()
                self_.es.close()
                return False

        return _Sc()

    def dk(self, key):
        if key not in self.dram_tk:
            self.dram_tk[key] = Tk()
        return self.dram_tk[key]

    @staticmethod
    def _tk(x):
        return x.k if isinstance(x, Buf) else x

    def _collect(self, e, R, Wr):
        toks = {}

        def add(tok):
            if e.need(tok):
                k = id(tok[0])
                if k not in toks or toks[k][1] < tok[1]:
                    toks[k] = tok

        for t in R:
            add(self._tk(t).w)
            if getattr(t, "psum", False):
                for tok in self._tk(t).r.values():
                    if tok[0] is not e.sem:
                        add(tok)
        for t in Wr:
            tk = self._tk(t)
            add(tk.w)
            for tok in tk.r.values():
                add(tok)
        return list(toks.values())

    def _finish(self, tok, R, Wr):
        for t in R:
            tk = self._tk(t)
            tk.r[id(tok[0])] = tok
        for t in Wr:
            tk = self._tk(t)
            tk.w = tok
            tk.r = {}

    mute = False

    def I(self, eng, fn, R=(), W=()):
        if self.mute:
            return None
        e = self.E[eng]
        if e.n >= 60000:
            e.sem = self.new_sem(e.name)
            e.n = 0
        toks = self._collect(e, R, W)
        for tok in toks[1:]:
            e.h.wait_ge(tok[0], tok[1])
            e.mark(tok)
            self.ninst += 1
        ins = fn(e.h)
        if toks:
            ins._wait_ge(toks[0][0], toks[0][1])
            e.mark(toks[0])
        e.n += 1
        ins.then_inc(e.sem, 1)
        self.ninst += 1
        self._finish((e.sem, e.n), R, W)
        return ins

    def dma(self, q, out, in_, R=(), W=()):
        if self.mute:
            return None
        e = self.E[q]
        ds = self.dsq[q][self.dnext[q]]
        self.dnext[q] = (self.dnext[q] + 1) % len(self.dsq[q])
        toks = self._collect(e, R, W)
        if ds[1] > 0 and e.need((ds[0], ds[1])):
            toks.append((ds[0], ds[1]))
        for tok in toks[1:]:
            e.h.wait_ge(tok[0], tok[1])
            e.mark(tok)
            self.ninst += 1
        ins = e.h.dma_start(out=out, in_=in_)
        if toks:
            ins._wait_ge(toks[0][0], toks[0][1])
            e.mark(toks[0])
        if ds[1] >= 60000:
            ds[0] = self.new_sem("d")
            ds[1] = 0
        ds[1] += 16
        ins.then_inc(ds[0], 16)
        self.ninst += 1
        self._finish((ds[0], ds[1]), R, W)
        return ins

    def final_wait(self, toks):
        e = self.E["sp"]
        for tok in toks:
            if tok is not None and e.need(tok):
                e.h.wait_ge(tok[0], tok[1])
                e.mark(tok)


import os
SKIP = set(os.environ.get('KSKIP', '').split(','))


def build(S, L, dbg=()):
    NT = S // 128
    NBK = S // 512
    TOPK = min(256, S // 4)
    nc = bass.Bass("TRN2", target_bir_lowering=False)
    kb = KB(nc)

    def din(name, shape, dt=F32):
        return nc.dram_tensor(name, list(shape), dt, kind="ExternalInput").ap()

    def dscr(name, shape, dt=F32):
        return nc.dram_tensor(name, list(shape), dt, kind="Internal").ap()

    x_in = din("x", [S, D])
    pos_in = din("pos", [128, NT], I32)
    wD = din("wD", [L, D, N_D])
    wA = din("wA", [L, D, WA_COLS])
    wB = din("wB", [L, D, N_B])
    mu_in = din("mu_in", [L, N_B])
    lora = din("lora", [L, 160, W])
    rowv = din("rowv", [L, NROW, W])
    out_d = nc.dram_tensor("out", [S, D], F32, kind="ExternalOutput").ap()
    pv_in = din("pv", [128, L, NPV])
    pvf_in = din("pvf", [128, NCH])
    wG = din("wG", [L, D, N_G])
    wC = din("wC", [L, D, N_C])
    pool_w = din("pool_w", [L, 4, 128, 128])
    w_branch = din("w_branch", [L, NB, W, D])
    w_out = din("w_out", [L, D, D])
    mlp_up = din("mlp_up", [L, D, 4 * D])
    mlp_down = din("mlp_down", [L, 4 * D, D])

    wG_b = dscr("wG_b", [L, D, N_G], BF16)
    wC_b = dscr("wC_b", [L, D, N_C], BF16)
    wbr_b = dscr("wbr_b", [L, NB, W, D], BF16)
    wout_b = dscr("wout_b", [L, D, D], BF16)
    wup_b = dscr("wup_b", [L, D, 4 * D], BF16)
    wdn_b = dscr("wdn_b", [L, 4 * D, D], BF16)
    poolw_b = dscr("poolw_b", [L, 4, 128, 128], BF16)
    wD_b = dscr("wD_b", [L, D, N_D], BF16)
    wA_b = dscr("wA_b", [L, D, WA_COLS], BF16)
    wB_b = dscr("wB_b", [L, 2 * D, N_B], BF16)
    aT_d = dscr("aT_d", [64, 15, S], BF16)
    vaug_d = dscr("vaug_d", [128, NT, 130], BF16)
    wi_d = dscr("wi_d", [128, NT, 4], F32)

    xT = dscr("xT", [128, NCH, S])
    gatesT = dscr("gatesT", [128, 32, S], BF16)
    oT = [dscr("o%dT" % i, [128, 4, S], BF16) for i in range(4)]

    dbg_out = {}
    for name in dbg:
        if name == "xT":
            dbg_out[name] = nc.dram_tensor("dbg_xT", [128, NCH, S], F32, kind="ExternalOutput").ap()
        elif name.startswith("o") and name.endswith("T"):
            dbg_out[name] = nc.dram_tensor("dbg_" + name, [128, 4, S], BF16, kind="ExternalOutput").ap()
        elif name == "gatesT":
            dbg_out[name] = nc.dram_tensor("dbg_gatesT", [128, 32, S], BF16, kind="ExternalOutput").ap()

    I = kb.I
    dma = kb.dma

    psA = [kb.ps("psA%d" % i, [128, 512]) for i in range(3)]
    ps_i = [0]

    def pbank():
        t = psA[ps_i[0] % len(psA)]
        ps_i[0] += 1
        return t

    ps_n = kb.ps("ps_n", [128, 512])
    ps_t = kb.ps("ps_t", [128, 1024], BF16)
    ps_f = kb.ps("ps_f", [128, 512])


    ident_f = kb.sb("ident_f", [128, 128], F32)
    ident_b = kb.sb("ident_b", [128, 128], BF16)
    ones_b = kb.sb("ones_b", [128, 128], BF16)
    I("pool", lambda h: h.memset(ident_f[:], 1.0), W=[ident_f])
    I("pool", lambda h: h.affine_select(out=ident_f[:], in_=ident_f[:], pattern=[[-1, 128]], compare_op=ALU.is_equal,
                                        fill=0.0, base=0, channel_multiplier=1), R=[ident_f], W=[ident_f])
    I("pool", lambda h: h.tensor_copy(out=ident_b[:], in_=ident_f[:]), R=[ident_f], W=[ident_b])
    I("pool", lambda h: h.memset(ones_b[:], 1.0), W=[ones_b])
    eps_t = kb.sb("eps_t", [128, 1], F32)
    I("pool", lambda h: h.memset(eps_t[:], NORM_EPS), W=[eps_t])

    pv = kb.sb("pv_sb", [128, L, NPV], F32)
    g_fin = kb.sb("g_fin", [128, NCH], F32)
    dma("sp", pv[:], pv_in, W=[pv])
    dma("sp", g_fin[:], pvf_in, W=[g_fin])
    g_attn = g_mlp = gb_sb = pscale = pv

    pm_cur = kb.sb("pm_cur", [128, 4, 128], BF16)
    pm_prev = kb.sb("pm_prev", [128, 4, 128], BF16)
    pm_first = kb.sb("pm_first", [128, 4, 128], BF16)
    pm_tmp = kb.sb("pm_tmp", [128, 128], F32)
    pm_tmp2 = kb.sb("pm_tmp2", [128, 128], F32)
    for gi, win in enumerate(POOL_WINDOWS):
        I("pool", lambda h: h.memset(pm_tmp[:], 1.0 / win), W=[pm_tmp])
        I("pool", lambda h: h.affine_select(out=pm_tmp[:], in_=pm_tmp[:], pattern=[[1, 128]], compare_op=ALU.is_ge,
                                            fill=0.0, base=0, channel_multiplier=-1), R=[pm_tmp], W=[pm_tmp])
        I("pool", lambda h: h.affine_select(out=pm_tmp[:], in_=pm_tmp[:], pattern=[[-1, 128]], compare_op=ALU.is_ge,
                                            fill=0.0, base=win - 1, channel_multiplier=1), R=[pm_tmp], W=[pm_tmp])
        I("pool", lambda h: h.tensor_tensor(out=pm_tmp2[:], in0=pm_tmp[:], in1=ident_f[:], op=ALU.subtract),
          R=[pm_tmp, ident_f], W=[pm_tmp2])
        I("pool", lambda h: h.tensor_copy(out=pm_cur[:, gi, :], in_=pm_tmp2[:]), R=[pm_tmp2], W=[pm_cur])
        for t in range(win - 1):
            I("pool", lambda h: h.memset(pm_tmp[0:t + 1, t:t + 1], 1.0 / (t + 1)), R=[pm_tmp], W=[pm_tmp])
        I("pool", lambda h: h.tensor_tensor(out=pm_tmp2[:], in0=pm_tmp[:], in1=ident_f[:], op=ALU.subtract),
          R=[pm_tmp, ident_f], W=[pm_tmp2])
        I("pool", lambda h: h.tensor_copy(out=pm_first[:, gi, :], in_=pm_tmp2[:]), R=[pm_tmp2], W=[pm_first])
        I("pool", lambda h: h.memset(pm_tmp[:], 1.0 / win), W=[pm_tmp])
        I("pool", lambda h: h.affine_select(out=pm_tmp[:], in_=pm_tmp[:], pattern=[[-1, 128]], compare_op=ALU.is_ge,
                                            fill=0.0, base=win - 129, channel_multiplier=1), R=[pm_tmp], W=[pm_tmp])
        I("pool", lambda h: h.tensor_copy(out=pm_prev[:, gi, :], in_=pm_tmp[:]), R=[pm_tmp], W=[pm_prev])

    NANG = 48
    cs_d = dscr("cs_d", [128, NT, NANG])
    with kb.scope():
      if "cs" not in SKIP:
        cs_tab = kb.sb("cs_tab", [128, NT, NANG], F32)
        pos_i = kb.sb("pos_i", [128, NT], I32)
        posf = kb.sb("posf", [128, NT], F32)
        ang = kb.sb("ang", [128, NT, NANG], F32)
        ang_i = kb.sb("ang_i", [128, NT, NANG], I32)
        ang_k = kb.sb("ang_k", [128, NT, NANG], F32)
        ang_r = kb.sb("ang_r", [128, NT, NANG], F32)
        dma("sp", pos_i[:], pos_in, W=[pos_i])
        I("dve", lambda h: h.tensor_copy(out=posf[:], in_=pos_i[:]), R=[pos_i], W=[posf])
        fa = [500000.0 ** (-(2.0 * i) / 16.0) for i in range(8)]
        fr = [1.0 / (10000.0 ** (i / 15.0)) for i in range(16)]
        cols = [(f, math.pi / 2) for f in fa] + [(f, 0.0) for f in fa] + [(f, math.pi / 2) for f in fr] + [(f, 0.0) for f in fr]
        for ci, (f, ph) in enumerate(cols):
            I("dve", lambda h: h.tensor_scalar(out=ang[:, :, ci], in0=posf[:], scalar1=float(np.float32(f)), scalar2=ph,
                                               op0=ALU.mult, op1=ALU.add), R=[posf], W=[ang])
        I("dve", lambda h: h.tensor_scalar(out=ang_i[:], in0=ang[:], scalar1=1.0 / (2 * math.pi), scalar2=None, op0=ALU.mult),
          R=[ang], W=[ang_i])
        I("dve", lambda h: h.tensor_copy(out=ang_k[:], in_=ang_i[:]), R=[ang_i], W=[ang_k])
        I("dve", lambda h: h.scalar_tensor_tensor(out=ang_r[:], in0=ang_k[:], scalar=-2 * math.pi, in1=ang[:], op0=ALU.mult, op1=ALU.add),
          R=[ang_k, ang], W=[ang_r])
        I("dve", lambda h: h.tensor_scalar(out=ang_k[:], in0=ang_r[:], scalar1=math.pi, scalar2=-2 * math.pi, op0=ALU.is_gt, op1=ALU.mult),
          R=[ang_r], W=[ang_k])
        I("dve", lambda h: h.tensor_tensor(out=ang[:], in0=ang_r[:], in1=ang_k[:], op=ALU.add), R=[ang_r, ang_k], W=[ang])
        I("dve", lambda h: h.tensor_scalar(out=ang_k[:], in0=ang[:], scalar1=-math.pi, scalar2=2 * math.pi, op0=ALU.is_lt, op1=ALU.mult),
          R=[ang], W=[ang_k])
        I("dve", lambda h: h.tensor_tensor(out=ang_r[:], in0=ang[:], in1=ang_k[:], op=ALU.add), R=[ang, ang_k], W=[ang_r])
        I("act", lambda h: h.activation(out=cs_tab[:], in_=ang_r[:], func=AF.Sin), R=[ang_r], W=[cs_tab])
        dma("sp", cs_d, cs_tab[:], R=[cs_tab], W=[kb.dk(("cs", 0))])

    RS = 32.0 ** -0.5
    lg = [math.log(1.0 - 2.0 ** (-5.0 - hh)) for hh in range(8)]
    r_intra = kb.sb("r_intra", [128, 8, 128], F32)
    r_qd = kb.sb("r_qd", [128, 4, 128], F32)
    r_kdec = kb.sb("r_kdec", [128, 8], F32)
    r_cdec = kb.sb("r_cdec", [128, 4], F32)
    bdmask = kb.sb("bdmask", [128, 4, 128], F32)
    I("pool", lambda h: h.memset(bdmask[:], 0.0), W=[bdmask])
    I("pool", lambda h: h.memset(bdmask[0:64, :, 0:64], 1.0), W=[bdmask])
    I("pool", lambda h: h.memset(bdmask[64:128, :, 64:128], 1.0), W=[bdmask])
    with kb.scope():
        kb.mute = "rconst" in SKIP
        dmat = kb.sb("dmat", [128, 128], F32)
        nmat = kb.sb("nmat", [128, 128], F32)
        mcol = kb.sb("mcol", [128, 1], F32)
        bias_t = kb.sb("bias_t", [128, 24], F32)
        ustr = kb.sb("ustr", [128, 128], BF16)
        I("pool", lambda h: h.memset(ustr[:], 1.0), W=[ustr])
        I("pool", lambda h: h.affine_select(out=ustr[:], in_=ustr[:], pattern=[[1, 128]], compare_op=ALU.is_gt,
                                            fill=0.0, base=0, channel_multiplier=-1), R=[ustr], W=[ustr])
        I("pe", lambda h: h.matmul(ps_f[:, 0:128], lhsT=ones_b[:], rhs=ustr[:], start=True, stop=True), R=[ones_b, ustr], W=[ps_f])
        I("pe", lambda h: h.matmul(ps_f[:, 128:129], lhsT=ustr[:], rhs=ones_b[:, 0:1], start=True, stop=True), R=[ones_b, ustr], W=[ps_f])
        I("dve", lambda h: h.tensor_copy(out=nmat[:], in_=ps_f[:, 0:128]), R=[ps_f], W=[nmat])
        I("dve", lambda h: h.tensor_copy(out=mcol[:], in_=ps_f[:, 128:129]), R=[ps_f], W=[mcol])
        I("dve", lambda h: h.tensor_scalar(out=dmat[:], in0=nmat[:], scalar1=mcol[:, 0:1], scalar2=None, op0=ALU.subtract),
          R=[nmat, mcol], W=[dmat])
        for hh in range(8):
            I("pool", lambda h: h.memset(bias_t[:, hh:hh + 1], math.log(RS)), W=[bias_t])
            I("pool", lambda h: h.memset(bias_t[:, 8 + hh:9 + hh], lg[hh] + math.log(RS)), W=[bias_t])
            I("pool", lambda h: h.memset(bias_t[:, 16 + hh:17 + hh], 127.0 * lg[hh]), W=[bias_t])
        for hh in range(8):
            I("act", lambda h: h.activation(out=r_intra[:, hh, :], in_=dmat[:], func=AF.Exp, scale=lg[hh], bias=bias_t[:, hh:hh + 1]),
              R=[dmat, bias_t], W=[r_intra])
            c, hf = hh // 2, hh % 2
            I("act", lambda h: h.activation(out=r_qd[hf * 64:(hf + 1) * 64, c, :], in_=nmat[hf * 64:(hf + 1) * 64, :], func=AF.Exp,
                                            scale=lg[hh], bias=bias_t[hf * 64:(hf + 1) * 64, 8 + hh:9 + hh]), R=[nmat, bias_t], W=[r_qd])
            I("act", lambda h: h.activation(out=r_kdec[:, hh:hh + 1], in_=mcol[:], func=AF.Exp, scale=-lg[hh],
                                            bias=bias_t[:, 16 + hh:17 + hh]), R=[mcol, bias_t], W=[r_kdec])
            I("pool", lambda h: h.memset(r_cdec[hf * 64:(hf + 1) * 64, c:c + 1], math.exp(128.0 * lg[hh])), W=[r_cdec])
        I("pool", lambda h: h.affine_select(out=r_intra[:], in_=r_intra[:], pattern=[[0, 8], [1, 128]], compare_op=ALU.is_ge,
                                            fill=0.0, base=0, channel_multiplier=-1), R=[r_intra], W=[r_intra])

    kb.mute = False
    def cast_w(dst, src, key, nsplit):
        n = src.shape[0]
        step = n // nsplit
        for i in range(nsplit):
            dma("pool", dst[i * step:(i + 1) * step], src[i * step:(i + 1) * step], W=[kb.dk(key)])

    for l in range(L):
        cast_w(wG_b[l], wG[l], ("wG", l), 4)
        cast_w(wC_b[l], wC[l], ("wC", l), 1)
        cast_w(wD_b[l], wD[l], ("wD", l), 2)
        cast_w(wA_b[l], wA[l], ("wA", l), 2)
        cast_w(wbr_b[l].rearrange("i k n -> (i k) n"), w_branch[l].rearrange("i k n -> (i k) n"), ("wbr", l), 2)
        cast_w(wout_b[l], w_out[l], ("wout", l), 1)
        cast_w(wup_b[l], mlp_up[l], ("wup", l), 4)
        cast_w(wdn_b[l], mlp_down[l], ("wdn", l), 4)
        cast_w(poolw_b[l].rearrange("g c d -> (g c) d"), pool_w[l].rearrange("g c d -> (g c) d"), ("poolw", l), 1)

    with kb.scope():
        mu_bc = kb.sb("mu_bc", [128, N_B], F32)
        omu_bc = kb.sb("omu_bc", [128, N_B], F32)
        wst = [kb.sb("wst%d" % i, [128, N_B], F32) for i in range(2)]
        wlo = [kb.sb("wlo%d" % i, [128, N_B], BF16) for i in range(2)]
        whi = [kb.sb("whi%d" % i, [128, N_B], BF16) for i in range(2)]
        for l in range(L):
            dma("sp", mu_bc[:], mu_in[l, :].partition_broadcast(128), W=[mu_bc])
            I("dve", lambda h: h.tensor_scalar(out=omu_bc[:], in0=mu_bc[:], scalar1=-1.0, scalar2=1.0, op0=ALU.mult, op1=ALU.add),
              R=[mu_bc], W=[omu_bc])
            for c in range(NCH):
                ws, wl, wh = wst[c % 2], wlo[c % 2], whi[c % 2]
                dma("sp", ws[:], wB[l, c * 128:(c + 1) * 128, :], W=[ws])
                I("dve", lambda h: h.tensor_tensor(out=wl[:], in0=ws[:], in1=omu_bc[:], op=ALU.mult), R=[ws, omu_bc], W=[wl])
                I("pool", lambda h: h.tensor_tensor(out=wh[:], in0=ws[:], in1=mu_bc[:], op=ALU.mult), R=[ws, mu_bc], W=[wh])
                dma("sp", wB_b[l, c * 128:(c + 1) * 128, :], wl[:], R=[wl], W=[kb.dk(("wB", l))])
                dma("sp", wB_b[l, D + c * 128:D + (c + 1) * 128, :], wh[:], R=[wh], W=[kb.dk(("wB", l))])

    xin = kb.sb("xin", [128, NCH, 512], F32)
    sq = kb.sb("sq", [128, NCH, 512], BF16)
    rstd = kb.sb("rstd", [128, 512], F32)
    hTb = kb.sb("hT", [128, NCH, 520], BF16)
    hT = HView(hTb)
    NWT = 4
    wts = []

    def alloc_wts():
        wts[:] = [kb.sb("wt%d" % i, [128, 4096], BF16) for i in range(NWT)]
    wt_i = [0]

    def wtile():
        t = wts[wt_i[0] % NWT]
        wt_i[0] += 1
        return t

    def norm_block(g_tile, gsel):
        I("act", lambda h: h.activation(out=sq[:].rearrange("p c t -> p (c t)"), in_=xin[:].rearrange("p c t -> p (c t)"),
                                        func=AF.Square), R=[xin], W=[sq])
        for c in range(NCH):
            I("pe", lambda h: h.matmul(ps_n[:], lhsT=ones_b[:], rhs=sq[:, c, :], start=(c == 0), stop=(c == NCH - 1)),
              R=[ones_b, sq], W=[ps_n])
        I("act", lambda h: h.activation(out=rstd[:], in_=ps_n[:], func=AF.Sqrt, bias=eps_t[:, 0:1], scale=1.0 / D),
          R=[ps_n, eps_t], W=[rstd])
        I("dve", lambda h: h.reciprocal(out=rstd[:], in_=rstd[:]), R=[rstd], W=[rstd])
        for c in range(NCH):
            I("dve", lambda h: h.scalar_tensor_tensor(out=hT[:, c, :], in0=xin[:, c, :], scalar=gsel(c), in1=rstd[:],
                                                       op0=ALU.mult, op1=ALU.mult), R=[xin, rstd, g_tile], W=[hT])

    def group_norm(src_ps, o_sb, xc, sqt, stat, eps):
        I("act", lambda h: h.activation(out=o_sb[:], in_=src_ps[:], func=AF.Copy), R=[src_ps], W=[o_sb])
        o3_ = o_sb[:].rearrange("p (s d) -> p s d", d=64)
        x3_ = xc[:].rearrange("p (s d) -> p s d", d=64)
        s3_ = sqt[:].rearrange("p (s d) -> p s d", d=64)
        I("dve", lambda h: h.tensor_reduce(out=stat[:, 0:8], in_=o3_, axis=AX.X, op=ALU.add), R=[o_sb], W=[stat])
        I("dve", lambda h: h.tensor_scalar(out=stat[:, 8:16], in0=stat[:, 0:8], scalar1=1.0 / 64, scalar2=None, op0=ALU.mult), R=[stat], W=[stat])
        I("dve", lambda h: h.tensor_tensor(out=x3_, in0=o3_, in1=stat[:, 8:16].unsqueeze(2).to_broadcast([128, 8, 64]), op=ALU.subtract),
          R=[o_sb, stat], W=[xc])
        I("act", lambda h: h.activation(out=sqt[:], in_=xc[:], func=AF.Square), R=[xc], W=[sqt])
        I("dve", lambda h: h.tensor_reduce(out=stat[:, 16:24], in_=s3_, axis=AX.X, op=ALU.add), R=[sqt], W=[stat])
        I("dve", lambda h: h.tensor_scalar(out=stat[:, 16:24], in0=stat[:, 16:24], scalar1=1.0 / 64, scalar2=eps, op0=ALU.mult, op1=ALU.add),
          R=[stat], W=[stat])
        I("act", lambda h: h.activation(out=stat[:, 24:32], in_=stat[:, 16:24], func=AF.Sqrt), R=[stat], W=[stat])
        I("dve", lambda h: h.reciprocal(out=stat[:, 24:32], in_=stat[:, 24:32]), R=[stat], W=[stat])
        I("dve", lambda h: h.tensor_tensor(out=x3_, in0=x3_, in1=stat[:, 24:32].unsqueeze(2).to_broadcast([128, 8, 64]), op=ALU.mult),
          R=[xc, stat], W=[xc])

    with kb.scope():
        xtok = kb.sb("xtok", [128, D], F32)
        for t in range(NT):
            blk = t // 4
            sub = t % 4
            dma("sp", xtok[:], x_in[t * 128:(t + 1) * 128, :], W=[xtok])
            for half in range(2):
                for c4 in range(4):
                    c = half * 4 + c4
                    I("pe", lambda h: h.transpose(out=ps_f[:, c4 * 128:(c4 + 1) * 128], in_=xtok[:, c * 128:(c + 1) * 128],
                                                  identity=ident_f[:]), R=[xtok, ident_f], W=[ps_f])
                I("act" if half == 0 else "dve",
                  lambda h: (h.activation(out=xin[:, half * 4:(half + 1) * 4, sub * 128:(sub + 1) * 128],
                                          in_=ps_f[:].rearrange("p (c t) -> p c t", c=4), func=AF.Copy) if half == 0 else
                             h.tensor_copy(out=xin[:, half * 4:(half + 1) * 4, sub * 128:(sub + 1) * 128],
                                           in_=ps_f[:].rearrange("p (c t) -> p c t", c=4))),
                  R=[ps_f], W=[xin])
            if sub == 3:
                dma("sp", xT[:, :, blk * 512:(blk + 1) * 512], xin[:], R=[xin], W=[kb.dk(("xT", blk))])

    def phaseA(l):
        if "pha" in SKIP:
            return
        alloc_wts()
        pc_prev = kb.sb("pc_prev", [128, 512], BF16)
        pc_cur = [kb.sb("pc_cur%d" % i, [128, 512], BF16) for i in range(2)]
        pooledT = kb.sb("pooledT", [128, 4, 128], BF16)
        ocT_blk = kb.sb("ocT_blk", [128, 4, 512], BF16)
        gts = [kb.sb("gts%d" % i, [128, 512], BF16) for i in range(2)]
        zero_blk = kb.sb("zero_blk", [128, 4, 512], BF16)
        I("pool", lambda h: h.memset(zero_blk[:], 0.0), W=[zero_blk])
        poolw_sb = kb.sb("poolw_sb", [128, 4, 128], BF16)
        wC_sb = kb.sb("wC_sb", [128, NCH, 512], BF16)
        wD_sb = kb.sb("wD_sb", [128, NCH, N_D], BF16)
        wA_sb = kb.sb("wA_sb", [128, NCH, WA_COLS], BF16)
        cs_blk = kb.sb("cs_blk", [128, 4, 48], F32)
        dma("sp", wA_sb[:], wA_b[l].rearrange("(c p) n -> p c n", p=128), R=[kb.dk(("wA", l))], W=[wA_sb])
        arot_b = kb.sb("arot_b", [128, 960], BF16)
        at1 = kb.sb("at1", [128, 128], F32)
        at2 = kb.sb("at2", [128, 128], F32)
        aT_blk = kb.sb("aT_blk", [64, 15, 512], BF16)
        vaug_blk = kb.sb("vaug_blk", [128, 4, 130], BF16)
        wi_blk = kb.sb("wi_blk", [128, 4, 4], F32)
        I("pool", lambda h: h.memset(vaug_blk[:], 1.0), W=[vaug_blk])
        gng_bc = kb.sb("gng_bc", [128, W], F32)
        rqk_z = kb.sb("rqk_z", [128, 1024], BF16)
        kdk_z = kb.sb("kdk_z", [128, 512], BF16)
        I("pool", lambda h: h.memset(rqk_z[:], 0.0), W=[rqk_z])
        I("pool", lambda h: h.memset(kdk_z[:], 0.0), W=[kdk_z])
        rv_b = kb.sb("rv_b", [128, 512], BF16)
        rsil = kb.sb("rsil", [128, 512], F32)
        rt1 = kb.sb("rt1", [128, 256], F32)
        rt2 = kb.sb("rt2", [128, 256], F32)
        rT_sb = kb.sb("rT_sb", [128, 8, 128], BF16)
        qd_b = kb.sb("qd_b", [128, 4, 128], BF16)
        scm_b = kb.sb("scm_b", [128, 8, 128], BF16)
        rstate = kb.sb("rstate", [128, 4, 128], F32)
        rstate_b = kb.sb("rstate_b", [128, 4, 128], BF16)
        qbd = kb.sb("qbd", [128, 4, 256], BF16)
        I("pool", lambda h: h.memset(qbd[:], 0.0), W=[qbd])
        ro_sb = kb.sb("ro_sb", [128, 512], F32)
        ro_xc = kb.sb("ro_xc", [128, 512], F32)
        ro_sq = kb.sb("ro_sq", [128, 512], F32)
        gstat = kb.sb("gstat", [128, 32], F32)
        od_tm = kb.sb("od_tm", [128, 512], BF16)
        odT_blk = kb.sb("odT_blk", [128, 4, 512], BF16)
        dma("sp", wD_sb[:], wD_b[l].rearrange("(c p) n -> p c n", p=128), R=[kb.dk(("wD", l))], W=[wD_sb])
        dma("sp", gng_bc[:], rowv[l, RV_GNG, :].partition_broadcast(128), W=[gng_bc])
        I("pool", lambda h: h.memset(rstate[:], 0.0), W=[rstate])
        I("pool", lambda h: h.memset(rstate_b[:], 0.0), W=[rstate_b])
        dma("sp", wC_sb[:], wC_b[l].rearrange("(c p) n -> p c n", p=128), R=[kb.dk(("wC", l))], W=[wC_sb])
        dma("sp", poolw_sb[:], poolw_b[l].rearrange("g c d -> c g d"), R=[kb.dk(("poolw", l))], W=[poolw_sb])
        for blk in range(NBK):
            dma("sp", xin[:], xT[:, :, blk * 512:(blk + 1) * 512], R=[kb.dk(("xT", blk))], W=[xin])
            norm_block(g_attn, lambda c: pv[:, l, PV_AG + c:PV_AG + c + 1])
            dma("sp", cs_blk[:], cs_d[:, blk * 4:(blk + 1) * 4, :], R=[kb.dk(("cs", 0))], W=[cs_blk])
            for gq in range(8):
                wt = wtile()
                dma("sp", wt[:].rearrange("p (c n) -> p c n", c=NCH),
                    wG_b[l, :, gq * 512:(gq + 1) * 512].rearrange("(c p) n -> p c n", p=128),
                    R=[kb.dk(("wG", l))], W=[wt])
                wv = wt[:].rearrange("p (c n) -> p c n", c=NCH)
                for j in range(4):
                    ch = gq * 4 + j
                    pb = pbank()
                    for c in range(NCH):
                        I("pe", lambda h: h.matmul(pb[:], lhsT=wv[:, c, j * 128:(j + 1) * 128], rhs=hT[:, c, :],
                                                   start=(c == 0), stop=(c == NCH - 1)), R=[wt, hT], W=[pb])
                    gt = gts[ch % 2]
                    I("act", lambda h: h.activation(out=gt[:], in_=pb[:], func=AF.Sigmoid, bias=pv[:, l, PV_GB + ch:PV_GB + ch + 1]),
                      R=[pb, gb_sb], W=[gt])
                    dma("sp", gatesT[:, ch, blk * 512:(blk + 1) * 512], gt[:], R=[gt], W=[kb.dk(("gatesT", blk))])
            for sub in range(4):
                t = blk * 4 + sub
                tsl = slice(sub * 128, (sub + 1) * 128)
                kb.mute = "apro" in SKIP
                pa0 = pbank()
                pa1 = pbank()
                pa2 = pbank()
                for bi, (pbk, c0, c1) in enumerate(((pa0, 0, 512), (pa1, 512, 1024), (pa2, 1024, WA_COLS))):
                    for c in range(NCH):
                        I("pe", lambda h: h.matmul(pbk[:, 0:c1 - c0], lhsT=hT[:, c, tsl], rhs=wA_sb[:, c, c0:c1],
                                                   start=(c == 0), stop=(c == NCH - 1)), R=[hT, wA_sb], W=[pbk])
                kb.mute = kb.mute or ("apc" in SKIP)
                I("act", lambda h: h.activation(out=arot_b[:, 0:512], in_=pa0[:], func=AF.Copy), R=[pa0], W=[arot_b])
                I("act", lambda h: h.activation(out=arot_b[:, 512:960], in_=pa1[:, 0:448], func=AF.Copy), R=[pa1], W=[arot_b])
                kb.mute = kb.mute or ("ap0" in SKIP)
                for (pbk, ns, so) in ((pa0, 8, 0), (pa1, 7, 8)):
                    p3 = pbk[:, 0:ns * 64].rearrange("p (s d) -> p s d", d=64)
                    x1, x2 = p3[:, :, 0:8], p3[:, :, 8:16]
                    cosb = cs_blk[:, sub, 0:8].unsqueeze(1).to_broadcast([128, ns, 8])
                    sinb = cs_blk[:, sub, 8:16].unsqueeze(1).to_broadcast([128, ns, 8])
                    o3a = arot_b[:].rearrange("p (s d) -> p s d", d=64)[:, so:so + ns, :]
                    b1 = at1[:, 0:ns * 8].rearrange("p (s d) -> p s d", d=8)
                    b2 = at2[:, 0:ns * 8].rearrange("p (s d) -> p s d", d=8)
                    I("dve", lambda h: h.tensor_tensor(out=b1, in0=x1, in1=cosb, op=ALU.mult), R=[pbk, cs_tab, arot_b], W=[at1])
                    I("dve", lambda h: h.tensor_tensor(out=b2, in0=x2, in1=sinb, op=ALU.mult), R=[pbk, cs_blk], W=[at2])
                    I("dve", lambda h: h.tensor_tensor(out=o3a[:, :, 0:8], in0=b1, in1=b2, op=ALU.subtract), R=[at1, at2], W=[arot_b])
                    I("dve", lambda h: h.tensor_tensor(out=b1, in0=x2, in1=cosb, op=ALU.mult), R=[pbk, cs_blk], W=[at1])
                    I("dve", lambda h: h.tensor_tensor(out=b2, in0=x1, in1=sinb, op=ALU.mult), R=[pbk, cs_blk], W=[at2])
                    I("dve", lambda h: h.tensor_tensor(out=o3a[:, :, 8:16], in0=b1, in1=b2, op=ALU.add), R=[at1, at2], W=[arot_b])
                kb.mute = kb.mute or ("ap1" in SKIP)
                kb.mute = kb.mute or ("ap1" in SKIP)
                I("act", lambda h: h.activation(out=vaug_blk[:, sub, :].rearrange("p (g d) -> p g d", g=2)[:, :, 0:64],
                                                in_=pa2[:, 0:128].rearrange("p (g d) -> p g d", g=2), func=AF.Copy), R=[pa2], W=[vaug_blk])
                I("act", lambda h: h.activation(out=wi_blk[:, sub, :], in_=pa2[:, 128:132], func=AF.Copy), R=[pa2], W=[wi_blk])
                kb.mute = kb.mute or ("ap2" in SKIP)
                for s0 in (0, 8):
                    ns = min(8, 15 - s0)
                    for si in range(ns):
                        I("pe", lambda h: h.transpose(out=ps_t[0:64, si * 128:(si + 1) * 128], in_=arot_b[:, (s0 + si) * 64:(s0 + si + 1) * 64],
                                                      identity=ident_b[:]), R=[arot_b, ident_b], W=[ps_t])
                    I("act", lambda h: h.activation(out=aT_blk[:, s0:s0 + ns, tsl], in_=ps_t[0:64, 0:ns * 128].rearrange("p (s t) -> p s t", t=128),
                                                    func=AF.Copy), R=[ps_t], W=[aT_blk])
                kb.mute = False
                pb = pbank()
                for c in range(NCH):
                    I("pe", lambda h: h.matmul(pb[:], lhsT=hT[:, c, tsl], rhs=wC_sb[:, c, :], start=(c == 0), stop=(c == NCH - 1)),
                      R=[hT, wC_sb], W=[pb])
                pcc = pc_cur[t % 2]
                pcp = pc_cur[(t + 1) % 2]
                I("act", lambda h: h.activation(out=pcc[:], in_=pb[:], func=AF.Copy), R=[pb], W=[pcc])
                pb2 = pbank()
                for gi in range(4):
                    mc = pm_first if t == 0 else pm_cur
                    I("pe", lambda h: h.matmul(pb2[:, gi * 128:(gi + 1) * 128], lhsT=pcc[:, gi * 128:(gi + 1) * 128], rhs=mc[:, gi, :],
                                               start=True, stop=(t == 0)), R=[pcc, mc], W=[pb2])
                    if t > 0:
                        I("pe", lambda h: h.matmul(pb2[:, gi * 128:(gi + 1) * 128], lhsT=pcp[:, gi * 128:(gi + 1) * 128], rhs=pm_prev[:, gi, :],
                                                   start=False, stop=True), R=[pcp, pm_prev], W=[pb2])
                I("dve", lambda h: h.tensor_copy(out=pooledT[:].rearrange("p g t -> p (g t)"), in_=pb2[:]), R=[pb2], W=[pooledT])
                pb3 = pbank()
                for gi in range(4):
                    I("pe", lambda h: h.matmul(pb3[:, gi * 128:(gi + 1) * 128], lhsT=poolw_sb[:, gi, :], rhs=pooledT[:, gi, :],
                                               start=True, stop=True), R=[poolw_sb, pooledT], W=[pb3])
                for gi in range(4):
                    I("act", lambda h: h.activation(out=ocT_blk[:, gi, tsl], in_=pb3[:, gi * 128:(gi + 1) * 128], func=AF.Copy,
                                                    scale=pv[:, l, PV_PS + gi:PV_PS + gi + 1]), R=[pb3, pscale], W=[ocT_blk])
                kb.mute = "ret" in SKIP
                pq = pbank()
                pv_ = pbank()
                pg = pbank()
                for bi, pbk in enumerate((pq, pv_, pg)):
                    for c in range(NCH):
                        I("pe", lambda h: h.matmul(pbk[:], lhsT=hT[:, c, tsl], rhs=wD_sb[:, c, bi * 512:(bi + 1) * 512],
                                                   start=(c == 0), stop=(c == NCH - 1)), R=[hT, wD_sb], W=[pbk])
                pq3 = pq[:].rearrange("p (s d) -> p s d", d=32)
                x1, x2 = pq3[:, :, 0:16], pq3[:, :, 16:32]
                cosb = cs_blk[:, sub, 16:32].unsqueeze(1).to_broadcast([128, 16, 16])
                sinb = cs_blk[:, sub, 32:48].unsqueeze(1).to_broadcast([128, 16, 16])
                o3 = rqk_z[:].rearrange("p (s d) -> p s d", d=64)
                a1 = rt1[:].rearrange("p (s d) -> p s d", d=16)
                a2 = rt2[:].rearrange("p (s d) -> p s d", d=16)
                _m = kb.mute
                kb.mute = _m or ("r_rot" in SKIP)
                I("dve", lambda h: h.tensor_tensor(out=a1, in0=x1, in1=cosb, op=ALU.mult), R=[pq, cs_blk], W=[rt1])
                I("dve", lambda h: h.tensor_tensor(out=a2, in0=x2, in1=sinb, op=ALU.mult), R=[pq, cs_blk], W=[rt2])
                I("dve", lambda h: h.tensor_tensor(out=o3[:, :, 0:16], in0=a1, in1=a2, op=ALU.subtract), R=[rt1, rt2], W=[rqk_z])
                I("dve", lambda h: h.tensor_tensor(out=a1, in0=x2, in1=cosb, op=ALU.mult), R=[pq, cs_blk], W=[rt1])
                I("dve", lambda h: h.tensor_tensor(out=a2, in0=x1, in1=sinb, op=ALU.mult), R=[pq, cs_blk], W=[rt2])
                I("dve", lambda h: h.tensor_tensor(out=o3[:, :, 16:32], in0=a1, in1=a2, op=ALU.add), R=[rt1, rt2], W=[rqk_z])
                kb.mute = _m or ("r_cp" in SKIP)
                I("act", lambda h: h.activation(out=rv_b[:], in_=pv_[:], func=AF.Copy), R=[pv_], W=[rv_b])
                kb.mute = _m or ("r_sil" in SKIP)
                I("act", lambda h: h.activation(out=rsil[:], in_=pg[:], func=AF.Silu), R=[pg], W=[rsil])
                kb.mute = _m or ("r_kdk" in SKIP)
                I("dve", lambda h: h.tensor_tensor(out=kdk_z[:].rearrange("p (s d) -> p s d", d=64),
                                                   in0=o3[:, 8:16, :],
                                                   in1=r_kdec[:].unsqueeze(2).to_broadcast([128, 8, 64]), op=ALU.mult),
                  R=[rqk_z, r_kdec], W=[kdk_z])
                kb.mute = _m or ("ret2" in SKIP)
                for c8 in range(8):
                    I("pe", lambda h: h.transpose(out=ps_t[:, c8 * 128:(c8 + 1) * 128], in_=rqk_z[:, c8 * 128:(c8 + 1) * 128],
                                                  identity=ident_b[:]), R=[rqk_z, ident_b], W=[ps_t])
                I("act", lambda h: h.activation(out=rT_sb[:].rearrange("p c t -> p (c t)"), in_=ps_t[:], func=AF.Copy),
                  R=[ps_t], W=[rT_sb])
                I("dve", lambda h: h.tensor_tensor(out=qd_b[:], in0=rT_sb[:, 0:4, :], in1=r_qd[:], op=ALU.mult), R=[rT_sb, r_qd], W=[qd_b])
                kb.mute = kb.mute or ("ret4" in SKIP)
                I("dve", lambda h: h.tensor_copy(out=qbd[0:64, :, 0:128], in_=rT_sb[0:64, 0:4, :]), R=[rT_sb], W=[qbd])
                I("dve", lambda h: h.tensor_copy(out=qbd[64:128, :, 128:256], in_=rT_sb[64:128, 0:4, :]), R=[rT_sb], W=[qbd])
                psc = [pbank(), pbank()]
                for c in range(4):
                    I("pe", lambda h: h.matmul(psc[c // 2][:, (c % 2) * 256:(c % 2 + 1) * 256], lhsT=rT_sb[:, 4 + c, :], rhs=qbd[:, c, :],
                                               start=True, stop=True), R=[rT_sb, qbd], W=[psc[c // 2]])
                for c2 in range(2):
                    I("dve", lambda h: h.tensor_tensor(out=scm_b[:, c2 * 4:(c2 + 1) * 4, :], in0=psc[c2][:].rearrange("p (s n) -> p s n", n=128),
                                                       in1=r_intra[:, c2 * 4:(c2 + 1) * 4, :], op=ALU.mult), R=[psc[c2], r_intra], W=[scm_b])
                kb.mute = kb.mute or ("r5" in SKIP)
                po = pbank()
                for c in range(4):
                    I("pe", lambda h: h.matmul(po[:, c * 128:(c + 1) * 128], lhsT=qd_b[:, c, :], rhs=rstate_b[:, c, :],
                                               start=True, stop=False), R=[qd_b, rstate_b], W=[po])
                    for j in range(2):
                        hh = 2 * c + j
                        I("pe", lambda h: h.matmul(po[:, hh * 64:(hh + 1) * 64], lhsT=scm_b[:, hh, :], rhs=rv_b[:, hh * 64:(hh + 1) * 64],
                                                   start=False, stop=(j == 1)), R=[scm_b, rv_b], W=[po])
                kb.mute = kb.mute or ("r6" in SKIP)
                pst = pbank()
                for c in range(4):
                    I("pe", lambda h: h.matmul(pst[:, c * 128:(c + 1) * 128], lhsT=kdk_z[:, c * 128:(c + 1) * 128],
                                               rhs=rv_b[:, c * 128:(c + 1) * 128], start=True, stop=True), R=[kdk_z, rv_b], W=[pst])
                kb.mute = kb.mute or ("r7" in SKIP)
                I("dve", lambda h: h.tensor_tensor(out=ro_sq[:], in0=pst[:], in1=bdmask[:].rearrange("p c d -> p (c d)"), op=ALU.mult),
                  R=[pst, bdmask], W=[ro_sq])
                I("dve", lambda h: h.tensor_tensor(out=rstate[:], in0=rstate[:], in1=r_cdec[:].unsqueeze(2).to_broadcast([128, 4, 128]), op=ALU.mult),
                  R=[rstate, r_cdec], W=[rstate])
                I("dve", lambda h: h.tensor_tensor(out=rstate[:].rearrange("p c d -> p (c d)"), in0=rstate[:].rearrange("p c d -> p (c d)"),
                                                   in1=ro_sq[:], op=ALU.add), R=[rstate, ro_sq], W=[rstate])
                I("dve", lambda h: h.tensor_copy(out=rstate_b[:], in_=rstate[:]), R=[rstate], W=[rstate_b])
                kb.mute = kb.mute or ("ret3" in SKIP)
                group_norm(po, ro_sb, ro_xc, ro_sq, gstat, RET_GN_EPS)
                I("dve", lambda h: h.tensor_tensor(out=ro_xc[:], in0=ro_xc[:], in1=gng_bc[:], op=ALU.mult), R=[ro_xc, gng_bc], W=[ro_xc])
                I("dve", lambda h: h.tensor_tensor(out=od_tm[:], in0=ro_xc[:], in1=rsil[:], op=ALU.mult), R=[ro_xc, rsil], W=[od_tm])
                for c4 in range(4):
                    I("pe", lambda h: h.transpose(out=ps_t[:, c4 * 128:(c4 + 1) * 128], in_=od_tm[:, c4 * 128:(c4 + 1) * 128],
                                                  identity=ident_b[:]), R=[od_tm, ident_b], W=[ps_t])
                I("act", lambda h: h.activation(out=odT_blk[:, :, tsl], in_=ps_t[:, 0:512].rearrange("p (c t) -> p c t", c=4), func=AF.Copy),
                  R=[ps_t], W=[odT_blk])
            kb.mute = False
            dma("sp", oT[2][:, :, blk * 512:(blk + 1) * 512], ocT_blk[:], R=[ocT_blk], W=[kb.dk(("o2T", blk))])
            dma("sp", oT[3][:, :, blk * 512:(blk + 1) * 512], odT_blk[:], R=[odT_blk], W=[kb.dk(("o3T", blk))])
            dma("sp", aT_d[:, :, blk * 512:(blk + 1) * 512], aT_blk[:], R=[aT_blk], W=[kb.dk(("aT", blk))])
            dma("sp", vaug_d[:, blk * 4:(blk + 1) * 4, :], vaug_blk[:], R=[vaug_blk], W=[kb.dk(("vaug", blk))])
            dma("sp", wi_d[:, blk * 4:(blk + 1) * 4, :], wi_blk[:], R=[wi_blk], W=[kb.dk(("wi", blk))])

    def phaseB(l):
        if "rwkv" in SKIP:
            return
        CD = math.exp(-0.5)
        psX = [kb.ps("psX0", [128, 512]), kb.ps("psX1", [128, 512])]
        psA.extend(psX)
        f32t = lambda n: kb.sb(n, [128, 512], F32)
        b16t = lambda n: kb.sb(n, [128, 512], BF16)
        wB_sb = kb.sb("wB_sb", [128, 16, N_B], BF16)
        dma("sp", wB_sb[:], wB_b[l].rearrange("(c p) n -> p c n", p=128), R=[kb.dk(("wB", l))], W=[wB_sb])
        bc = {}
        for nm, ri in (("w0", RV_W0), ("a0", RV_A0), ("kk", RV_KK), ("ka", RV_KA), ("rk", RV_RK), ("lng", RV_LNG), ("lnb", RV_LNB)):
            bc[nm] = f32t("bc_" + nm)
            dma("sp", bc[nm][:], rowv[l, ri, :].partition_broadcast(128), W=[bc[nm]])
        lora_b = kb.sb("lora_b", [96, 3, W], BF16)
        with kb.scope():
            lora_f = kb.sb("lora_f", [96, 3, W], F32)
            I("pool", lambda h: h.memset(lora_f[:], 0.0), W=[lora_f])
            dma("sp", lora_f[0:32, 0, :], lora[l, 0:32, :], W=[lora_f])
            dma("sp", lora_f[0:32, 1, :], lora[l, 32:64, :], W=[lora_f])
            dma("sp", lora_f[0:96, 2, :], lora[l, 64:160, :], W=[lora_f])
            I("dve", lambda h: h.tensor_copy(out=lora_b[:], in_=lora_f[:]), R=[lora_f], W=[lora_b])
        Lm = kb.sb("Lm", [128, 3, 128], F32)
        negc = kb.sb("negc", [128, 1], F32)
        msk = kb.sb("msk", [128, 3, 128], BF16)
        I("pool", lambda h: h.memset(Lm[:], -CD), W=[Lm])
        I("pool", lambda h: h.memset(negc[:], -CD), W=[negc])
        I("pool", lambda h: h.memset(msk[:], 1.0), W=[msk])
        for (tile_, j, pat, cm, op) in ((Lm, 0, 1, -1, ALU.is_ge), (Lm, 1, 1, -1, ALU.is_gt), (Lm, 2, -1, 1, ALU.is_gt),
                                        (msk, 0, 1, -1, ALU.is_gt), (msk, 1, 1, -1, ALU.is_ge), (msk, 2, -1, 1, ALU.is_gt)):
            I("pool", lambda h: h.affine_select(out=tile_[:, j, :], in_=tile_[:, j, :], pattern=[[pat, 128]], compare_op=op,
                                                fill=0.0, base=0, channel_multiplier=cm), R=[tile_], W=[tile_])
        r_f, k_f, v_f, sgw, a_f, g_f, kkf, kpf, b_f, tA, tB = [f32t(n) for n in ("r_f", "k_f", "v_f", "sgw", "a_f", "g_f", "kkf", "kpf", "b_f", "tA", "tB")]
        Ep, En, Eex, Erev = [f32t(n) for n in ("Ep", "En", "Eex", "Erev")]
        al_tm, be_tm, ka_tm, rh_tm, bp_tm, kp_tm, v_b, U_b, ob_tm = [b16t(n) for n in
            ("al_tm", "be_tm", "ka_tm", "rh_tm", "bp_tm", "kp_tm", "v_b", "U_b", "ob_tm")]
        XT = kb.sb("XT", [128, 4, 4, 128], BF16)
        Xbd = kb.sb("Xbd", [128, 3, 4, 256], BF16)
        I("pool", lambda h: h.memset(Xbd[:], 0.0), W=[Xbd])
        M5 = kb.sb("M5", [128, 5, 8, 128], BF16)
        CHD = F32
        chA = [kb.sb("chA%d" % i, [128, 4, 128], CHD) for i in range(2)]
        chAT = [kb.sb("chAT%d" % i, [128, 4, 128], CHD) for i in range(2)]
        chP = [kb.sb("chP%d" % i, [128, 4, 128], CHD) for i in range(2)]
        MT = kb.sb("MT", [128, 8, 128], CHD)
        RHS_c = kb.sb("RHS_c", [128, 512], CHD)
        ST_f = kb.sb("ST_f", [128, 4, 128], F32)
        ST_b = kb.sb("ST_b", [128, 4, 128], BF16)
        I("pool", lambda h: h.memset(ST_f[:], 0.0), W=[ST_f])
        I("pool", lambda h: h.memset(ST_b[:], 0.0), W=[ST_b])
        st8 = kb.sb("st8", [128, 40], F32)
        dC = kb.sb("dC", [128, 4], F32)
        gstat = kb.sb("gstatb", [128, 32], F32)
        lw = kb.sb("lw", [96, 3, 512], BF16)
        obT_blk = kb.sb("obT_blk", [128, 4, 512], BF16)
        I("pool", lambda h: h.memset(hTb[:], 0.0), W=[hTb])
        for blk in range(NBK):
            dma("sp", xin[:], xT[:, :, blk * 512:(blk + 1) * 512], R=[kb.dk(("xT", blk))], W=[xin])
            if blk > 0:
                I("dve", lambda h: h.tensor_copy(out=hTb[:, :, 7:8], in_=hTb[:, :, 519:520]), R=[hTb], W=[hTb])
            I("act", lambda h: h.activation(out=sq[:].rearrange("p c t -> p (c t)"), in_=xin[:].rearrange("p c t -> p (c t)"),
                                            func=AF.Square), R=[xin], W=[sq])
            for c in range(NCH):
                I("pe", lambda h: h.matmul(ps_n[:], lhsT=ones_b[:], rhs=sq[:, c, :], start=(c == 0), stop=(c == NCH - 1)),
                  R=[ones_b, sq], W=[ps_n])
            I("act", lambda h: h.activation(out=rstd[:], in_=ps_n[:], func=AF.Sqrt, bias=eps_t[:, 0:1], scale=1.0 / D),
              R=[ps_n, eps_t], W=[rstd])
            I("dve", lambda h: h.reciprocal(out=rstd[:], in_=rstd[:]), R=[rstd], W=[rstd])
            for c in range(NCH):
                I("dve", lambda h: h.scalar_tensor_tensor(out=hTb[:, c, 8:520], in0=xin[:, c, :], scalar=pv[:, l, PV_AG + c:PV_AG + c + 1],
                                                           in1=rstd[:], op0=ALU.mult, op1=ALU.mult), R=[xin, rstd, pv], W=[hTb])
            for gi_, (c0, cn, fn_) in enumerate(((1536, 32, AF.Tanh), (1568, 32, AF.Copy), (1600, 96, AF.Sigmoid))):
                pb = pbank()
                for c in range(16):
                    off = 8 if c < 8 else 7
                    I("pe", lambda h: h.matmul(pb[0:cn, :], lhsT=wB_sb[:, c, c0:c0 + cn], rhs=hTb[:, c % 8, off:off + 512],
                                               start=(c == 0), stop=(c == 15)), R=[wB_sb, hTb], W=[pb])
                I("act", lambda h: h.activation(out=lw[0:cn, gi_, :], in_=pb[0:cn, :], func=fn_), R=[pb], W=[lw])
            for sub in range(4):
                t = blk * 4 + sub
                tsl = slice(sub * 128, (sub + 1) * 128)
                for gi_, dst in enumerate((r_f, k_f, v_f)):
                    pb = pbank()
                    for c in range(16):
                        off = (8 if c < 8 else 7) + sub * 128
                        I("pe", lambda h: h.matmul(pb[:], lhsT=hTb[:, c % 8, off:off + 128], rhs=wB_sb[:, c, gi_ * 512:(gi_ + 1) * 512],
                                                   start=(c == 0), stop=(c == 15)), R=[wB_sb, hTb], W=[pb])
                    I("act", lambda h: h.activation(out=dst[:], in_=pb[:], func=AF.Copy), R=[pb], W=[dst])
                I("act", lambda h: h.activation(out=v_b[:], in_=v_f[:], func=AF.Copy), R=[v_f], W=[v_b])
                pzw = pbank()
                I("pe", lambda h: h.matmul(pzw[:], lhsT=lw[0:32, 0, tsl], rhs=lora_b[0:32, 0, :], start=True, stop=True), R=[lw, lora_b], W=[pzw])
                I("dve", lambda h: h.tensor_tensor(out=tA[:], in0=pzw[:], in1=bc["w0"][:], op=ALU.add), R=[pzw, bc["w0"]], W=[tA])
                I("act", lambda h: h.activation(out=sgw[:], in_=tA[:], func=AF.Sigmoid), R=[tA], W=[sgw])
                pza = pbank()
                I("pe", lambda h: h.matmul(pza[:], lhsT=lw[0:32, 1, tsl], rhs=lora_b[0:32, 1, :], start=True, stop=True), R=[lw, lora_b], W=[pza])
                I("dve", lambda h: h.tensor_tensor(out=tB[:], in0=pza[:], in1=bc["a0"][:], op=ALU.add), R=[pza, bc["a0"]], W=[tB])
                I("act", lambda h: h.activation(out=a_f[:], in_=tB[:], func=AF.Sigmoid), R=[tB], W=[a_f])
                pg_ = pbank()
                I("pe", lambda h: h.matmul(pg_[:], lhsT=lw[0:96, 2, tsl], rhs=lora_b[0:96, 2, :], start=True, stop=True), R=[lw, lora_b], W=[pg_])
                I("act", lambda h: h.activation(out=g_f[:], in_=pg_[:], func=AF.Copy), R=[pg_], W=[g_f])
                v3 = lambda tl: tl[:].rearrange("p (s d) -> p s d", d=64)
                I("pool", lambda h: h.tensor_tensor(out=kkf[:], in0=k_f[:], in1=bc["kk"][:], op=ALU.mult), R=[k_f, bc["kk"]], W=[kkf])
                I("act", lambda h: h.activation(out=tA[:], in_=kkf[:], func=AF.Square), R=[kkf], W=[tA])
                I("dve", lambda h: h.tensor_reduce(out=st8[:, 0:8], in_=v3(tA), axis=AX.X, op=ALU.add), R=[tA], W=[st8])
                I("act", lambda h: h.activation(out=st8[:, 8:16], in_=st8[:, 0:8], func=AF.Sqrt), R=[st8], W=[st8])
                I("dve", lambda h: h.tensor_scalar(out=st8[:, 8:16], in0=st8[:, 8:16], scalar1=1e-12, scalar2=None, op0=ALU.max), R=[st8], W=[st8])
                I("dve", lambda h: h.reciprocal(out=st8[:, 16:24], in_=st8[:, 8:16]), R=[st8], W=[st8])
                I("dve", lambda h: h.tensor_tensor(out=v3(kkf), in0=v3(kkf), in1=st8[:, 16:24].unsqueeze(2).to_broadcast([128, 8, 64]), op=ALU.mult),
                  R=[kkf, st8], W=[kkf])
                I("dve", lambda h: h.scalar_tensor_tensor(out=tB[:], in0=a_f[:], scalar=-1.0, in1=bc["ka"][:], op0=ALU.add, op1=ALU.mult),
                  R=[a_f, bc["ka"]], W=[tB])
                I("dve", lambda h: h.scalar_tensor_tensor(out=kpf[:], in0=tB[:], scalar=1.0, in1=k_f[:], op0=ALU.add, op1=ALU.mult),
                  R=[tB, k_f], W=[kpf])
                I("dve", lambda h: h.tensor_tensor(out=b_f[:], in0=kkf[:], in1=a_f[:], op=ALU.mult), R=[kkf, a_f], W=[b_f])
                I("pool", lambda h: h.tensor_tensor(out=tA[:], in0=r_f[:], in1=kpf[:], op=ALU.mult), R=[r_f, kpf], W=[tA])
                I("pool", lambda h: h.tensor_tensor(out=tA[:], in0=tA[:], in1=bc["rk"][:], op=ALU.mult), R=[tA, bc["rk"]], W=[tA])
                I("dve", lambda h: h.tensor_reduce(out=st8[:, 24:32], in_=v3(tA), axis=AX.X, op=ALU.add), R=[tA], W=[st8])
                pcs = []
                for j in range(3):
                    pb = pbank()
                    I("pe", lambda h: h.matmul(pb[:], lhsT=Lm[:, j, :], rhs=sgw[:], start=True, stop=True), R=[Lm, sgw], W=[pb])
                    pcs.append(pb)
                I("act", lambda h: h.activation(out=Ep[:], in_=pcs[0][:], func=AF.Exp), R=[pcs[0]], W=[Ep])
                I("act", lambda h: h.activation(out=En[:], in_=pcs[0][:], func=AF.Exp, scale=-1.0), R=[pcs[0]], W=[En])
                I("act", lambda h: h.activation(out=Eex[:], in_=pcs[1][:], func=AF.Exp), R=[pcs[1]], W=[Eex])
                I("act", lambda h: h.activation(out=Erev[:], in_=pcs[2][:], func=AF.Exp), R=[pcs[2]], W=[Erev])
                for p_ in range(4):
                    I("pe", lambda h: h.matmul(ps_f[:, p_:p_ + 1], lhsT=sgw[:, p_ * 128:(p_ + 1) * 128], rhs=negc[:], start=True, stop=True),
                      R=[sgw, negc], W=[ps_f])
                I("act", lambda h: h.activation(out=dC[:], in_=ps_f[:, 0:4], func=AF.Exp), R=[ps_f], W=[dC])
                I("dve", lambda h: h.tensor_tensor(out=al_tm[:], in0=kkf[:], in1=Eex[:], op=ALU.mult), R=[kkf, Eex], W=[al_tm])
                I("pool", lambda h: h.tensor_tensor(out=be_tm[:], in0=b_f[:], in1=En[:], op=ALU.mult), R=[b_f, En], W=[be_tm])
                I("dve", lambda h: h.tensor_tensor(out=ka_tm[:], in0=kpf[:], in1=En[:], op=ALU.mult), R=[kpf, En], W=[ka_tm])
                I("pool", lambda h: h.tensor_tensor(out=rh_tm[:], in0=r_f[:], in1=Ep[:], op=ALU.mult), R=[r_f, Ep], W=[rh_tm])
                I("dve", lambda h: h.tensor_tensor(out=bp_tm[:], in0=b_f[:], in1=Erev[:], op=ALU.mult), R=[b_f, Erev], W=[bp_tm])
                I("pool", lambda h: h.tensor_tensor(out=kp_tm[:], in0=kpf[:], in1=Erev[:], op=ALU.mult), R=[kpf, Erev], W=[kp_tm])
                for half, (ta, tb_) in enumerate(((al_tm, be_tm), (ka_tm, rh_tm))):
                    for xi, src in enumerate((ta, tb_)):
                        for p_ in range(4):
                            I("pe", lambda h: h.transpose(out=ps_t[:, (xi * 4 + p_) * 128:(xi * 4 + p_ + 1) * 128], in_=src[:, p_ * 128:(p_ + 1) * 128],
                                                          identity=ident_b[:]), R=[src, ident_b], W=[ps_t])
                    I("act", lambda h: h.activation(out=XT[:, half * 2:half * 2 + 2, :, :].rearrange("p x q t -> p (x q t)"), in_=ps_t[:], func=AF.Copy),
                      R=[ps_t], W=[XT])
                for bi_, xi in enumerate((0, 1, 3)):
                    I("dve", lambda h: h.tensor_copy(out=Xbd[0:64, bi_, :, 0:128], in_=XT[0:64, xi, :, :]), R=[XT], W=[Xbd])
                    I("pool", lambda h: h.tensor_copy(out=Xbd[64:128, bi_, :, 128:256], in_=XT[64:128, xi, :, :]), R=[XT], W=[Xbd])
                specs = ((1, 0, 0), (0, 1, 2), (2, 0, 0), (1, 2, 1), (2, 2, 1))
                for ti, (lt, rb, mk) in enumerate(specs):
                    for rnd in range(2):
                        pb = pbank()
                        for pp in range(2):
                            p_ = rnd * 2 + pp
                            I("pe", lambda h: h.matmul(pb[:, pp * 256:(pp + 1) * 256], lhsT=XT[:, lt, p_, :], rhs=Xbd[:, rb, p_, :],
                                                       start=True, stop=True), R=[XT, Xbd], W=[pb])
                        I("dve", lambda h: h.tensor_tensor(out=M5[:, ti, rnd * 4:(rnd + 1) * 4, :], in0=pb[:].rearrange("p (a t) -> p a t", t=128),
                                                           in1=msk[:, mk, :].unsqueeze(1).to_broadcast([128, 4, 128]), op=ALU.mult),
                          R=[pb, msk], W=[M5])
                for rnd in range(2):
                    hs = slice(rnd * 4, (rnd + 1) * 4)
                    A_, AT_ = M5[:, 1, hs, :], M5[:, 0, hs, :]
                    Abuf, ATbuf = M5, M5
                    I("dve", lambda h: h.tensor_tensor(out=chP[0][:], in0=ident_b[:].unsqueeze(1).to_broadcast([128, 4, 128]), in1=AT_, op=ALU.subtract),
                      R=[ident_b, M5], W=[chP[0]])
                    pcur = 0
                    for k_ in range(6):
                        pa_ = pbank()
                        for j in range(4):
                            I("pe", lambda h: h.matmul(pa_[:, j * 128:(j + 1) * 128], lhsT=AT_[:, j, :], rhs=A_[:, j, :], start=True, stop=True),
                              R=[Abuf, ATbuf], W=[pa_])
                        if k_ < 5:
                            pat_ = pbank()
                            for j in range(4):
                                I("pe", lambda h: h.matmul(pat_[:, j * 128:(j + 1) * 128], lhsT=A_[:, j, :], rhs=AT_[:, j, :], start=True, stop=True),
                                  R=[Abuf, ATbuf], W=[pat_])
                        nA = chA[k_ % 2]
                        I("act", lambda h: h.activation(out=nA[:].rearrange("p a t -> p (a t)"), in_=pa_[:], func=AF.Copy), R=[pa_], W=[nA])
                        if k_ < 5:
                            nAT = chAT[k_ % 2]
                            I("dve", lambda h: h.tensor_copy(out=nAT[:].rearrange("p a t -> p (a t)"), in_=pat_[:]), R=[pat_], W=[nAT])
                        pp_ = pbank()
                        for j in range(4):
                            I("pe", lambda h: h.matmul(pp_[:, j * 128:(j + 1) * 128], lhsT=nA[:, j, :], rhs=chP[pcur][:, j, :], start=True, stop=True),
                              R=[nA, chP[pcur]], W=[pp_])
                        dstP = MT[:, hs, :] if k_ == 5 else chP[1 - pcur][:]
                        dstB = MT if k_ == 5 else chP[1 - pcur]
                        I("dve", lambda h: h.tensor_tensor(out=dstP, in0=pp_[:].rearrange("p (a t) -> p a t", t=128), in1=chP[pcur][:], op=ALU.add),
                          R=[pp_, chP[pcur]], W=[dstB])
                        pcur = 1 - pcur
                        if k_ < 5:
                            A_, AT_ = nA[:], nAT[:]
                            Abuf, ATbuf = nA, nAT
                prhs = pbank()
                for p_ in range(4):
                    I("pe", lambda h: h.matmul(prhs[:, p_ * 128:(p_ + 1) * 128], lhsT=XT[:, 0, p_, :], rhs=ST_b[:, p_, :], start=True, stop=False),
                      R=[XT, ST_b], W=[prhs])
                    for j in range(2):
                        hd = p_ * 2 + j
                        I("pe", lambda h: h.matmul(prhs[:, hd * 64:(hd + 1) * 64], lhsT=M5[:, 2, hd, :], rhs=v_b[:, hd * 64:(hd + 1) * 64],
                                                   start=False, stop=(j == 1)), R=[M5, v_b], W=[prhs])
                I("act", lambda h: h.activation(out=RHS_c[:], in_=prhs[:], func=AF.Copy), R=[prhs], W=[RHS_c])
                pu = pbank()
                for hd in range(8):
                    I("pe", lambda h: h.matmul(pu[:, hd * 64:(hd + 1) * 64], lhsT=MT[:, hd, :], rhs=RHS_c[:, hd * 64:(hd + 1) * 64],
                                               start=True, stop=True), R=[MT, RHS_c], W=[pu])
                I("act", lambda h: h.activation(out=U_b[:], in_=pu[:], func=AF.Copy, scale=-1.0), R=[pu], W=[U_b])
                py = pbank()
                for p_ in range(4):
                    I("pe", lambda h: h.matmul(py[:, p_ * 128:(p_ + 1) * 128], lhsT=XT[:, 3, p_, :], rhs=ST_b[:, p_, :], start=True, stop=False),
                      R=[XT, ST_b], W=[py])
                    for j in range(2):
                        hd = p_ * 2 + j
                        I("pe", lambda h: h.matmul(py[:, hd * 64:(hd + 1) * 64], lhsT=M5[:, 3, hd, :], rhs=U_b[:, hd * 64:(hd + 1) * 64],
                                                   start=False, stop=False), R=[M5, U_b], W=[py])
                        I("pe", lambda h: h.matmul(py[:, hd * 64:(hd + 1) * 64], lhsT=M5[:, 4, hd, :], rhs=v_b[:, hd * 64:(hd + 1) * 64],
                                                   start=False, stop=(j == 1)), R=[M5, v_b], W=[py])
                pst = pbank()
                for p_ in range(4):
                    psl = slice(p_ * 128, (p_ + 1) * 128)
                    I("pe", lambda h: h.matmul(pst[:, psl], lhsT=bp_tm[:, psl], rhs=U_b[:, psl], start=True, stop=False), R=[bp_tm, U_b], W=[pst])
                    I("pe", lambda h: h.matmul(pst[:, psl], lhsT=kp_tm[:, psl], rhs=v_b[:, psl], start=False, stop=True), R=[kp_tm, v_b], W=[pst])
                I("dve", lambda h: h.tensor_tensor(out=tA[:], in0=pst[:], in1=bdmask[:].rearrange("p c d -> p (c d)"), op=ALU.mult),
                  R=[pst, bdmask], W=[tA])
                I("dve", lambda h: h.tensor_tensor(out=ST_f[:], in0=ST_f[:], in1=dC[:].unsqueeze(2).to_broadcast([128, 4, 128]), op=ALU.mult),
                  R=[ST_f, dC], W=[ST_f])
                I("dve", lambda h: h.tensor_tensor(out=ST_f[:].rearrange("p c d -> p (c d)"), in0=ST_f[:].rearrange("p c d -> p (c d)"), in1=tA[:], op=ALU.add),
                  R=[ST_f, tA], W=[ST_f])
                I("dve", lambda h: h.tensor_copy(out=ST_b[:], in_=ST_f[:]), R=[ST_f], W=[ST_b])
                group_norm(py, Ep, En, Eex, gstat, RWKV_GN_EPS)
                I("dve", lambda h: h.tensor_tensor(out=En[:], in0=En[:], in1=bc["lng"][:], op=ALU.mult), R=[En, bc["lng"]], W=[En])
                I("pool", lambda h: h.tensor_tensor(out=En[:], in0=En[:], in1=bc["lnb"][:], op=ALU.add), R=[En, bc["lnb"]], W=[En])
                I("dve", lambda h: h.tensor_tensor(out=v3(tB), in0=v3(v_f), in1=st8[:, 24:32].unsqueeze(2).to_broadcast([128, 8, 64]), op=ALU.mult),
                  R=[v_f, st8], W=[tB])
                I("pool", lambda h: h.tensor_tensor(out=En[:], in0=En[:], in1=tB[:], op=ALU.add), R=[En, tB], W=[En])
                I("dve", lambda h: h.tensor_tensor(out=ob_tm[:], in0=En[:], in1=g_f[:], op=ALU.mult), R=[En, g_f], W=[ob_tm])
                for c4 in range(4):
                    I("pe", lambda h: h.transpose(out=ps_t[:, c4 * 128:(c4 + 1) * 128], in_=ob_tm[:, c4 * 128:(c4 + 1) * 128],
                                                  identity=ident_b[:]), R=[ob_tm, ident_b], W=[ps_t])
                I("act", lambda h: h.activation(out=obT_blk[:, :, tsl], in_=ps_t[:, 0:512].rearrange("p (c t) -> p c t", c=4), func=AF.Copy),
                  R=[ps_t], W=[obT_blk])
            dma("sp", oT[1][:, :, blk * 512:(blk + 1) * 512], obT_blk[:], R=[obT_blk], W=[kb.dk(("o1T", blk))])
        del psA[3:]

    def phaseAttn(l):
        if "attn" in SKIP:
            return
        NIT = 14
        SCALE = 64.0 ** -0.5
        kT = kb.sb("kT", [64, 2, S], BF16)
        kiT = kb.sb("kiT", [64, S], BF16)
        vaug = kb.sb("vaug", [128, NT, 130], BF16)
        allk = [kb.dk(("aT", b)) for b in range(NBK)]
        dma("sp", kT[:], aT_d[:, 8:10, :], R=allk, W=[kT])
        dma("sp", kiT[:], aT_d[:, 14, :], R=allk, W=[kiT])
        dma("sp", vaug[:], vaug_d, R=[kb.dk(("vaug", b)) for b in range(NBK)], W=[vaug])
        idx = kb.sb("idx", [128, S], F32)
        maskb = kb.sb("maskb", [128, S], BF16)
        maskT = kb.sb("maskT", [128, NT, 128], BF16)
        qT = kb.sb("qT", [64, 8, 128], BF16)
        qiT = kb.sb("qiT", [64, 4, 128], BF16)
        wi_t = kb.sb("wi_t", [128, 4], F32)
        dg = kb.sb("dg", [128, 4, 128], BF16)
        relu4 = [kb.sb("relu4_%d" % i, [128, 512], BF16) for i in range(8)]
        ebuf = [kb.sb("ebuf%d" % i, [128, 512], BF16) for i in range(4)]
        embuf = [kb.sb("embuf%d" % i, [128, 512], BF16) for i in range(4)]
        bs = kb.sb("bs", [128, 16], F32)
        steps = kb.sb("steps", [128, 3 * NIT], F32)
        pw2 = kb.sb("pw2", [128, NIT], F32)
        osb = kb.sb("osb", [65, 512], F32)
        sel65 = kb.sb("sel65", [65, 64], F32)
        rden = kb.sb("rden", [64, 512], F32)
        onorm = kb.sb("onorm", [64, 512], BF16)
        pacc = [kb.ps("pacc0", [128, 512]), kb.ps("pacc1", [128, 512])]
        for k in range(NIT):
            I("pool", lambda h: h.memset(pw2[:, k:k + 1], 2.0 ** -(k + 1)), W=[pw2])
        cmask = kb.sb("cmask", [128, 128], F32)
        I("pool", lambda h: h.memset(cmask[:], 0.0), W=[cmask])
        I("pool", lambda h: h.affine_select(out=cmask[:], in_=cmask[:], pattern=[[-1, 128]], compare_op=ALU.is_ge,
                                            fill=-30000.0, base=0, channel_multiplier=1), R=[cmask], W=[cmask])
        I("pool", lambda h: h.memset(bs[:], 0.0), W=[bs])
        I("pool", lambda h: h.memset(bs[:, 10:11], TOPK - 0.5), W=[bs])
        I("pool", lambda h: h.memset(sel65[:], 0.0), W=[sel65])
        I("pool", lambda h: h.memset(sel65[64:65, :], 1.0), W=[sel65])
        qTs = [qT, kb.sb("qT2", [64, 8, 128], BF16)]
        qiTs = [qiT, kb.sb("qiT2", [64, 4, 128], BF16)]
        wis = [wi_t, kb.sb("wi_t2", [128, 4], F32)]
        dgs = [dg, kb.sb("dg2", [128, 4, 128], BF16)]
        LO = 3 if NIT % 2 == 0 else 9

        pv_banks = [psA[0], psA[1]]
        s1_banks = [psA[2], ps_n]
        s1_i = [0]

        def prep_gen(qt):
            nk = (qt + 1) * 128
            qsl = slice(qt * 128, (qt + 1) * 128)
            qT_, qiT_, wi_, dg_ = qTs[qt % 2], qiTs[qt % 2], wis[qt % 2], dgs[qt % 2]
            dma("sp", qT_[:], aT_d[:, 0:8, qsl], R=allk, W=[qT_])
            dma("sp", qiT_[:], aT_d[:, 10:14, qsl], R=allk, W=[qiT_])
            dma("sp", wi_[:], wi_d[:, qt, :], R=[kb.dk(("wi", qt // 4))], W=[wi_])
            yield
            for hh in range(4):
                I("dve", lambda h: h.tensor_scalar(out=dg_[:, hh, :], in0=ident_b[:], scalar1=wi_[:, hh:hh + 1], scalar2=None, op0=ALU.mult),
                  R=[ident_b, wi_], W=[dg_])
            yield
            nchunk = (nk + 511) // 512

            def idx_s1(kc):
                c0 = kc * 512
                ncol = min(512, nk - c0)
                rset = (kc % 2) * 4
                for hh in range(4):
                    pb = s1_banks[s1_i[0] % 2]
                    s1_i[0] += 1
                    I("pe", lambda h: h.matmul(pb[:, 0:ncol], lhsT=qiT_[:, hh, :], rhs=kiT[:, c0:c0 + ncol], start=True, stop=True),
                      R=[qiT_, kiT], W=[pb])
                    I("act", lambda h: h.activation(out=relu4[rset + hh][:, 0:ncol], in_=pb[:, 0:ncol], func=AF.Relu), R=[pb], W=[relu4[rset + hh]])

            def idx_d(kc):
                c0 = kc * 512
                ncol = min(512, nk - c0)
                rset = (kc % 2) * 4
                psum_h = ps_f
                for hh in range(4):
                    I("pe", lambda h: h.matmul(psum_h[:, 0:ncol], lhsT=dg_[:, hh, :], rhs=relu4[rset + hh][:, 0:ncol], start=(hh == 0), stop=(hh == 3)),
                      R=[dg_, relu4[rset + hh]], W=[psum_h])
                I("dve", lambda h: h.tensor_copy(out=idx[:, c0:c0 + ncol], in_=psum_h[:, 0:ncol]), R=[psum_h], W=[idx])

            idx_s1(0)
            yield
            for kc in range(nchunk):
                if kc + 1 < nchunk:
                    idx_s1(kc + 1)
                idx_d(kc)
                yield
            I("dve", lambda h: h.tensor_reduce(out=bs[:, 0:1], in_=idx[:, 0:nk], axis=AX.X, op=ALU.min), R=[idx], W=[bs])
            I("pool", lambda h: h.tensor_tensor(out=idx[:, qsl], in0=idx[:, qsl], in1=cmask[:], op=ALU.add), R=[idx, bs, cmask], W=[idx])
            I("dve", lambda h: h.tensor_reduce(out=bs[:, 1:2], in_=idx[:, 0:nk], axis=AX.X, op=ALU.max), R=[idx], W=[bs])
            yield
            I("dve", lambda h: h.tensor_scalar(out=bs[:, 6:7], in0=bs[:, 0:1], scalar1=-1e-3, scalar2=None, op0=ALU.add), R=[bs], W=[bs])
            I("dve", lambda h: h.tensor_tensor(out=bs[:, 2:3], in0=bs[:, 1:2], in1=bs[:, 6:7], op=ALU.subtract), R=[bs], W=[bs])
            I("dve", lambda h: h.tensor_scalar(out=bs[:, 2:3], in0=bs[:, 2:3], scalar1=1e-3, scalar2=None, op0=ALU.add), R=[bs], W=[bs])
            I("dve", lambda h: h.tensor_scalar(out=steps[:, 0:NIT], in0=pw2[:], scalar1=bs[:, 2:3], scalar2=None, op0=ALU.mult), R=[pw2, bs], W=[steps])
            I("dve", lambda h: h.tensor_scalar(out=steps[:, NIT:2 * NIT], in0=steps[:, 0:NIT], scalar1=-1.0, scalar2=None, op0=ALU.mult), R=[steps], W=[steps])
            I("dve", lambda h: h.tensor_scalar(out=steps[:, 2 * NIT:3 * NIT], in0=steps[:, 0:NIT], scalar1=2.0, scalar2=None, op0=ALU.mult), R=[steps], W=[steps])
            mcol = 3
            I("dve", lambda h: h.tensor_tensor(out=bs[:, mcol:mcol + 1], in0=bs[:, 6:7], in1=steps[:, 0:1], op=ALU.add), R=[bs, steps], W=[bs])
            yield
            for k in range(NIT):
                I("dve", lambda h: h.tensor_scalar(out=maskb[:, 0:nk], in0=idx[:, 0:nk], scalar1=bs[:, mcol:mcol + 1], scalar2=bs[:, 8:9],
                                                   op0=ALU.is_ge, op1=ALU.add, accum_out=bs[:, 4:5]), R=[idx, bs], W=[maskb, bs])
                last = (k == NIT - 1)
                pcol = k if last else (2 * NIT + k + 1)
                ncolm = (NIT + k) if last else (NIT + k + 1)
                I("dve", lambda h: h.tensor_scalar(out=bs[:, 5:6], in0=bs[:, 4:5], scalar1=bs[:, 10:11], scalar2=steps[:, pcol:pcol + 1],
                                                   op0=ALU.is_ge, op1=ALU.mult), R=[bs, steps], W=[bs])
                ncolx = 9 if mcol == 3 else 3
                I("dve", lambda h: h.scalar_tensor_tensor(out=bs[:, ncolx:ncolx + 1], in0=bs[:, 5:6], scalar=steps[:, ncolm:ncolm + 1],
                                                           in1=bs[:, mcol:mcol + 1], op0=ALU.add, op1=ALU.add), R=[bs, steps], W=[bs])
                mcol = ncolx
                yield
            assert mcol == LO

        def exhaust(gen):
            if gen is not None:
                for _ in gen:
                    pass

        def mask_stage(qt):
            nk = (qt + 1) * 128
            nkb = qt + 1
            I("dve", lambda h: h.tensor_scalar(out=maskb[:, 0:nk], in0=idx[:, 0:nk], scalar1=bs[:, LO:LO + 1], scalar2=None, op0=ALU.is_ge),
              R=[idx, bs], W=[maskb])
            for k0 in range(0, nkb, 8):
                nn = min(8, nkb - k0)
                for j in range(nn):
                    I("pe", lambda h: h.transpose(out=ps_t[:, j * 128:(j + 1) * 128], in_=maskb[:, (k0 + j) * 128:(k0 + j + 1) * 128],
                                                  identity=ident_b[:]), R=[maskb, ident_b], W=[ps_t])
                I("act", lambda h: h.activation(out=maskT[:, k0:k0 + nn, :].rearrange("p k t -> p (k t)"), in_=ps_t[:, 0:nn * 128], func=AF.Copy),
                  R=[ps_t], W=[maskT])

        def pv_stage(qt, filler):
            nkb = qt + 1
            qT_ = qTs[qt % 2]
            nst = 2 * nkb
            pbs = {}

            def pv_s(i):
                kbi, g = i // 2, i % 2
                pb = pv_banks[i % 2]
                I("pe", lambda h: h.matmul(pb[:], lhsT=kT[:, g, kbi * 128:(kbi + 1) * 128], rhs=qT_[:, g * 4:(g + 1) * 4, :], start=True, stop=True),
                  R=[kT, qT_], W=[pb])
                pbs[i] = pb

            def pv_e(i):
                kbi, g = i // 2, i % 2
                pb = pbs.pop(i)
                eb, em = ebuf[i % 4], embuf[i % 4]
                I("act", lambda h: h.activation(out=eb[:], in_=pb[:], func=AF.Exp, scale=SCALE), R=[pb], W=[eb])
                I("dve", lambda h: h.tensor_tensor(out=em[:].rearrange("p (j t) -> p j t", j=4), in0=eb[:].rearrange("p (j t) -> p j t", j=4),
                                                   in1=maskT[:, kbi, :].unsqueeze(1).to_broadcast([128, 4, 128]), op=ALU.mult),
                  R=[eb, maskT], W=[em])

            def pv_m(i):
                kbi, g = i // 2, i % 2
                em = embuf[i % 4]
                I("pe", lambda h: h.matmul(pacc[g][0:65, :], lhsT=vaug[:, kbi, g * 65:(g + 1) * 65], rhs=em[:],
                                           start=(kbi == 0), stop=(kbi == nkb - 1)), R=[vaug, em], W=[pacc[g]])

            for i in range(nst + 2):
                if i < nst:
                    pv_s(i)
                if 0 <= i - 1 < nst:
                    pv_e(i - 1)
                if 0 <= i - 2 < nst:
                    pv_m(i - 2)
                if filler is not None:
                    next(filler, None)

        def fin_stage(qt):
            qsl = slice(qt * 128, (qt + 1) * 128)
            for g in range(2):
                I("act", lambda h: h.activation(out=osb[:], in_=pacc[g][0:65, :], func=AF.Copy), R=[pacc[g]], W=[osb])
                I("pe", lambda h: h.matmul(ps_f[0:64, :], lhsT=sel65[:], rhs=osb[:], start=True, stop=True), R=[sel65, osb], W=[ps_f])
                I("dve", lambda h: h.reciprocal(out=rden[:], in_=ps_f[0:64, :]), R=[ps_f], W=[rden])
                I("dve", lambda h: h.tensor_tensor(out=onorm[:], in0=osb[0:64, :], in1=rden[:], op=ALU.mult), R=[osb, rden], W=[onorm])
                for j in range(4):
                    hd = g * 4 + j
                    dma("sp", oT[0][(hd % 2) * 64:(hd % 2) * 64 + 64, hd // 2, qsl], onorm[:, j * 128:(j + 1) * 128],
                        R=[onorm], W=[kb.dk(("o0T", qt // 4))])

        exhaust(prep_gen(0))
        for qt in range(NT):
            mask_stage(qt)
            nxt = prep_gen(qt + 1) if qt + 1 < NT else None
            pv_stage(qt, nxt)
            fin_stage(qt)
            exhaust(nxt)

    def phaseC(l):
        kb.mute = False
        if "phc" in SKIP:
            return
        alloc_wts()
        oin = [kb.sb("oin%d" % i, [128, 4, 512], BF16) for i in range(4)]
        gin = kb.sb("gin", [128, 32, 512], BF16)
        merged = kb.sb("merged", [128, NCH, 512], F32)
        merged_b = kb.sb("merged_b", [128, NCH, 512], BF16)
        actT = kb.sb("actT", [128, 32, 512], BF16)
        relu_t = [kb.sb("relu%d" % i, [128, 512], BF16) for i in range(2)]
        otok = kb.sb("otok", [128, D], F32)
        hTf = kb.sb("hTf", [128, NCH, 512], F32)
        for blk in range(NBK):
            bsl = slice(blk * 512, (blk + 1) * 512)
            dma("sp", xin[:], xT[:, :, bsl], R=[kb.dk(("xT", blk))], W=[xin])
            for i in range(4):
                dma("sp", oin[i][:], oT[i][:, :, bsl], R=[kb.dk(("o%dT" % i, blk))], W=[oin[i]])
            for q4 in range(4):
                dma("sp", gin[:, q4 * 8:(q4 + 1) * 8, :], gatesT[:, q4 * 8:(q4 + 1) * 8, bsl], R=[kb.dk(("gatesT", blk))], W=[gin])
            for i in range(4):
                wt = wtile()
                wv = wt[:].rearrange("p (c n) -> p c n", c=4)
                dma("sp", wv, wbr_b[l, i].rearrange("(c p) n -> p c n", p=128), R=[kb.dk(("wbr", l))], W=[wt])
                for oc in range(NCH):
                    pb = pbank()
                    for c in range(4):
                        I("pe", lambda h: h.matmul(pb[:], lhsT=wv[:, c, oc * 128:(oc + 1) * 128], rhs=oin[i][:, c, :],
                                                   start=(c == 0), stop=(c == 3)), R=[wt, oin[i]], W=[pb])
                    if i == 0:
                        I("dve", lambda h: h.tensor_tensor(out=merged[:, oc, :], in0=pb[:], in1=gin[:, i * 8 + oc, :], op=ALU.mult),
                          R=[pb, gin], W=[merged])
                    else:
                        I("dve", lambda h: h.tensor_tensor(out=relu_t[0][:], in0=pb[:], in1=gin[:, i * 8 + oc, :], op=ALU.mult),
                          R=[pb, gin], W=[relu_t[0]])
                        if i < 3:
                            I("pool", lambda h: h.tensor_tensor(out=merged[:, oc, :], in0=merged[:, oc, :], in1=relu_t[0][:], op=ALU.add),
                              R=[merged, relu_t[0]], W=[merged])
                        else:
                            I("pool", lambda h: h.tensor_tensor(out=merged_b[:, oc, :], in0=merged[:, oc, :], in1=relu_t[0][:], op=ALU.add),
                              R=[merged, relu_t[0]], W=[merged_b])
            for half in range(2):
                wt = wtile()
                wv = wt[:].rearrange("p (c n) -> p c n", c=NCH)
                dma("sp", wv, wout_b[l, :, half * 512:(half + 1) * 512].rearrange("(c p) n -> p c n", p=128),
                    R=[kb.dk(("wout", l))], W=[wt])
                for j in range(4):
                    oc = half * 4 + j
                    pb = pbank()
                    for c in range(NCH):
                        I("pe", lambda h: h.matmul(pb[:], lhsT=wv[:, c, j * 128:(j + 1) * 128], rhs=merged_b[:, c, :],
                                                   start=(c == 0), stop=(c == NCH - 1)), R=[wt, merged_b], W=[pb])
                    I("dve", lambda h: h.tensor_tensor(out=xin[:, oc, :], in0=xin[:, oc, :], in1=pb[:], op=ALU.add),
                      R=[pb, xin], W=[xin])
            norm_block(g_mlp, lambda c: pv[:, l, PV_MG + c:PV_MG + c + 1])
            for f8 in range(8):
                wt = wtile()
                wv = wt[:].rearrange("p (c n) -> p c n", c=NCH)
                dma("sp", wv, wup_b[l, :, f8 * 512:(f8 + 1) * 512].rearrange("(c p) n -> p c n", p=128),
                    R=[kb.dk(("wup", l))], W=[wt])
                for j in range(4):
                    f = f8 * 4 + j
                    pb = pbank()
                    for c in range(NCH):
                        I("pe", lambda h: h.matmul(pb[:], lhsT=wv[:, c, j * 128:(j + 1) * 128], rhs=hT[:, c, :],
                                                   start=(c == 0), stop=(c == NCH - 1)), R=[wt, hT], W=[pb])
                    rt = relu_t[1]
                    I("act", lambda h: h.activation(out=rt[:], in_=pb[:], func=AF.Relu), R=[pb], W=[rt])
                    I("pool", lambda h: h.tensor_tensor(out=actT[:, f, :], in0=rt[:], in1=rt[:], op=ALU.mult), R=[rt], W=[actT])
            for oc in range(NCH):
                wt = wtile()
                wv = wt[:].rearrange("p (c n) -> p c n", c=32)
                dma("sp", wv, wdn_b[l, :, oc * 128:(oc + 1) * 128].rearrange("(c p) n -> p c n", p=128),
                    R=[kb.dk(("wdn", l))], W=[wt])
                pb = pbank()
                for c in range(32):
                    I("pe", lambda h: h.matmul(pb[:], lhsT=wv[:, c, :], rhs=actT[:, c, :], start=(c == 0), stop=(c == 31)),
                      R=[wt, actT], W=[pb])
                I("dve", lambda h: h.tensor_tensor(out=xin[:, oc, :], in0=xin[:, oc, :], in1=pb[:], op=ALU.add),
                  R=[pb, xin], W=[xin])
            if l < L - 1:
                dma("sp", xT[:, :, bsl], xin[:], R=[xin], W=[kb.dk(("xT", blk))])
            else:
                if "xT" in dbg_out:
                    dma("sp", dbg_out["xT"][:, :, bsl], xin[:], R=[xin], W=[kb.dk(("dbgx", blk))])
                I("act", lambda h: h.activation(out=sq[:].rearrange("p c t -> p (c t)"), in_=xin[:].rearrange("p c t -> p (c t)"),
                                                func=AF.Square), R=[xin], W=[sq])
                for c in range(NCH):
                    I("pe", lambda h: h.matmul(ps_n[:], lhsT=ones_b[:], rhs=sq[:, c, :], start=(c == 0), stop=(c == NCH - 1)),
                      R=[ones_b, sq], W=[ps_n])
                I("act", lambda h: h.activation(out=rstd[:], in_=ps_n[:], func=AF.Sqrt, bias=eps_t[:, 0:1], scale=1.0 / D),
                  R=[ps_n, eps_t], W=[rstd])
                I("dve", lambda h: h.reciprocal(out=rstd[:], in_=rstd[:]), R=[rstd], W=[rstd])
                for c in range(NCH):
                    I("dve", lambda h: h.scalar_tensor_tensor(out=hTf[:, c, :], in0=xin[:, c, :], scalar=g_fin[:, c:c + 1], in1=rstd[:],
                                                               op0=ALU.mult, op1=ALU.mult), R=[xin, rstd, g_fin], W=[hTf])
                for sub in range(4):
                    t = blk * 4 + sub
                    for half in range(2):
                        for c4 in range(4):
                            c = half * 4 + c4
                            I("pe", lambda h: h.transpose(out=ps_f[:, c4 * 128:(c4 + 1) * 128], in_=hTf[:, c, sub * 128:(sub + 1) * 128],
                                                          identity=ident_f[:]), R=[hTf, ident_f], W=[ps_f])
                        I("act", lambda h: h.activation(out=otok[:, half * 512:(half + 1) * 512], in_=ps_f[:], func=AF.Copy),
                          R=[ps_f], W=[otok])
                    dma("sp", out_d[t * 128:(t + 1) * 128, :], otok[:], R=[otok], W=[kb.dk(("out", t))])

    for l in range(L):
        with kb.scope():
            phaseA(l)
        with kb.scope():
            phaseB(l)
        with kb.scope():
            phaseAttn(l)
        with kb.scope():
            phaseC(l)

    for name, ap in dbg_out.items():
        if name == "xT":
            continue
        src = {"gatesT": gatesT, "o0T": oT[0], "o1T": oT[1], "o2T": oT[2], "o3T": oT[3]}[name]
        keys = [k for k in kb.dram_tk if k[0] == name]
        dma("sp", ap, src, R=[kb.dram_tk[k] for k in keys], W=[kb.dk(("dbg", name))])

    kb.final_wait([tk.w for key, tk in kb.dram_tk.items() if key[0] in ("out", "dbg", "dbgx")])
    kb.es.close()
    build.ninst = kb.ninst
    return nc


def prep_inputs(inputs, b, L):
    f = lambda a: np.ascontiguousarray(np.asarray(a), dtype=np.float32)
    w_in = np.asarray(inputs["w_in"])
    m = {
        "x": f(inputs["x"][b]),
        "pos": np.ascontiguousarray(np.asarray(inputs["positions"][b]).astype(np.int32).reshape(-1, 128).T),
        "wD": f(w_in[:, :, N_A + N_B + N_C:N_A + N_B + N_C + N_D]),
        "wB": f(np.concatenate([w_in[:, :, 1092:1604], w_in[:, :, 1636:2148], w_in[:, :, 2148:2660], w_in[:, :, 1604:1636],
                                w_in[:, :, 2660:2692], w_in[:, :, 2692:2788]], axis=2)),
        "mu_in": f(np.concatenate([np.asarray(inputs["rwkv_mu"])[:, i:j] for i, j in ((0, 512), (544, 1056), (1056, 1568), (512, 544), (1568, 1600), (1600, 1696))], axis=1)),
        "lora": f(np.concatenate([np.asarray(inputs["rwkv_w2"]), np.asarray(inputs["rwkv_a2"]), np.asarray(inputs["rwkv_g2"])], axis=1)),
        "wA": f(np.concatenate([w_in[:, :, 0:512], w_in[:, :, 512:640], w_in[:, :, 768:1088],
                                np.zeros_like(w_in[:, :, 0:64]), w_in[:, :, 640:768], w_in[:, :, 1088:1092]], axis=2)),
        "wG": f(w_in[:, :, N_A + N_B + N_C + N_D:]),
        "wC": f(w_in[:, :, N_A + N_B:N_A + N_B + N_C]),
        "pool_w": f(inputs["pool_w"]),
        "w_branch": f(inputs["w_branch"]),
        "w_out": f(inputs["w_out"]),
        "mlp_up": f(inputs["mlp_up"]),
        "mlp_down": f(inputs["mlp_down"]),
    }
    pv = np.zeros((128, L, NPV), np.float32)
    for l in range(L):
        pv[:, l, PV_AG:PV_AG + 8] = colmajor(inputs["attn_norm_g"][l])
        pv[:, l, PV_MG:PV_MG + 8] = colmajor(inputs["mlp_norm_g"][l])
        pv[:, l, PV_GB:PV_GB + 32] = colmajor(np.asarray(inputs["gate_b"][l]).reshape(-1))
        pv[:, l, PV_PS:PV_PS + 4] = colmajor(inputs["pool_scale"][l])
    m["pv"] = pv
    rowv = np.zeros((L, NROW, W), np.float32)
    rowv[:, RV_GNG] = np.asarray(inputs["ret_gn_g"])
    for ri, nm in ((RV_W0, "rwkv_w0"), (RV_A0, "rwkv_a0"), (RV_KK, "rwkv_k_k"), (RV_KA, "rwkv_k_a"), (RV_RK, "rwkv_r_k"),
                   (RV_LNG, "rwkv_ln_g"), (RV_LNB, "rwkv_ln_b")):
        rowv[:, ri] = np.asarray(inputs[nm])
    m["rowv"] = rowv
    m["pvf"] = colmajor(inputs["final_norm_g"])
    return m


def kernel(**inputs):
    x = np.asarray(inputs["x"])
    B, S, _ = x.shape
    L = np.asarray(inputs["w_in"]).shape[0]
    nc = build(S, L)
    in_maps = [prep_inputs(inputs, b, L) for b in range(B)]
    res = run_bass_kernel_spmd(nc, in_maps, core_ids=list(range(B)))
    return np.stack([np.asarray(r["out"]) for r in res.results], axis=0).astype(np.float32)
```
